# Optimizing a Trainium2 kernel written in Bass

```python
import math
import jax, jax.numpy as jnp
from jax import lax
import numpy as np

D_MODEL = 1024
BATCH = 16
SEQ = 2048
DEPTH = 2

N_EVEN = (DEPTH + 1) // 2
N_ODD = DEPTH // 2
DN_ALPHA = (2.0 * DEPTH) ** 0.25
DN_BETA = (8.0 * DEPTH) ** -0.25
LN_EPS = 1e-5
NEG = -1e30
Q_BLOCK = 128

GM_WIDTH = D_MODEL // 2
GM_GROUPS = 4
GM_GROUP_DIM = GM_WIDTH // GM_GROUPS
GM_CHUNK = 128

MLA_HEADS = 8
MLA_NOPE = 64
MLA_ROPE = 32
MLA_V = 64
MLA_Q_RANK = D_MODEL // 4
MLA_KV_RANK = D_MODEL // 8
ROPE_BASE = 10000.0
AB_IN = 2 * GM_WIDTH + MLA_Q_RANK + MLA_KV_RANK + MLA_ROPE
AB_MIX = GM_WIDTH + MLA_HEADS * MLA_V
AB_SPLITS = (GM_WIDTH, 2 * GM_WIDTH, 2 * GM_WIDTH + MLA_Q_RANK,
             2 * GM_WIDTH + MLA_Q_RANK + MLA_KV_RANK)

NSA_HEADS = 16
NSA_GROUPS = 2
NSA_HPG = NSA_HEADS // NSA_GROUPS
NSA_DH = 64
NSA_CMP_LEN = 32
NSA_CMP_STRIDE = 16
NSA_CMP_HIDDEN = 256
NSA_SEL_LEN = 64
NSA_TOPK = 8
NSA_SEL_QBLOCK = 64
NSA_WINDOW = 512
NSA_BRANCHES = 3
NSA_KV = NSA_GROUPS * NSA_DH
C_MIX = NSA_HEADS * NSA_DH
C_IN = C_MIX + 6 * NSA_KV + NSA_BRANCHES * NSA_HEADS
NSA_SPLITS = tuple(C_MIX + i * NSA_KV for i in range(7))
NSA_FORCE = 1e4

MOE_GROUPS = 4
MOE_EPG = 8
MOE_EXPERTS = MOE_GROUPS * MOE_EPG
MOE_TOPK = 2
MOE_HIDDEN = D_MODEL // 4

kernel_name = 'hybrid_gmlp_mla_nsa_hmoe_deepnorm'


def layer_norm(x, g, b):
    xf = x.astype(jnp.float32)
    mu = xf.mean(-1, keepdims=True)
    var = jnp.square(xf - mu).mean(-1, keepdims=True)
    return ((xf - mu) * lax.rsqrt(var + LN_EPS) * g + b).astype(x.dtype)


def rms_norm(x, g):
    xf = x.astype(jnp.float32)
    return (xf * lax.rsqrt(jnp.mean(xf * xf, -1, keepdims=True) + LN_EPS) * g).astype(x.dtype)


def rope(x, pos):
    half = x.shape[-1] // 2
    freq = jnp.exp(-math.log(ROPE_BASE) * jnp.arange(half, dtype=jnp.float32) / half)
    ang = pos.astype(jnp.float32)[:, :, None, None] * freq
    cos, sin = jnp.cos(ang), jnp.sin(ang)
    x1 = x[..., :half].astype(jnp.float32)
    x2 = x[..., half:].astype(jnp.float32)
    return jnp.concatenate([x1 * cos - x2 * sin, x1 * sin + x2 * cos], -1).astype(x.dtype)


def gmlp_mix(u, v, ln_g, ln_b, w_s, b_s):
    B, S, _ = u.shape
    nc = S // GM_CHUNK
    vn = layer_norm(v, ln_g, ln_b).reshape(B, nc, GM_CHUNK, GM_GROUPS, GM_GROUP_DIM)
    causal = jnp.tril(jnp.ones((GM_CHUNK, GM_CHUNK), dtype=bool))
    ws = jnp.where(causal, w_s, 0.0).astype(vn.dtype)
    s = jnp.einsum('gts,bnsgc->bntgc', ws, vn) + jnp.transpose(b_s)[:, :, None]
    return u * s.reshape(B, S, GM_WIDTH)


def mla_mix(c_q, c_kv, k_r, pos, q_norm_g, kv_norm_g, w_uq, w_uk, w_uv):
    B, S, _ = c_q.shape
    q = (rms_norm(c_q, q_norm_g) @ w_uq).reshape(B, S, MLA_HEADS, MLA_NOPE + MLA_ROPE)
    q_nope = q[..., :MLA_NOPE]
    q_rope = rope(q[..., MLA_NOPE:], pos)
    ckv = rms_norm(c_kv, kv_norm_g)
    k_nope = (ckv @ w_uk).reshape(B, S, MLA_HEADS, MLA_NOPE)
    v = (ckv @ w_uv).reshape(B, S, MLA_HEADS, MLA_V)
    k_rope = rope(k_r[:, :, None, :], pos)[:, :, 0]
    scale = (MLA_NOPE + MLA_ROPE) ** -0.5
    outs = []
    for i in range(S // Q_BLOCK):
        q0, q1 = i * Q_BLOCK, (i + 1) * Q_BLOCK
        s = (jnp.einsum('bqhd,bkhd->bhqk', q_nope[:, q0:q1], k_nope[:, :q1])
             + jnp.einsum('bqhr,bkr->bhqk', q_rope[:, q0:q1], k_rope[:, :q1]))
        mask = (q0 + jnp.arange(Q_BLOCK))[:, None] >= jnp.arange(q1)[None, :]
        s = jnp.where(mask, s.astype(jnp.float32) * scale, NEG)
        p = jax.nn.softmax(s, axis=-1).astype(v.dtype)
        outs.append(jnp.einsum('bhqk,bkhd->bqhd', p, v[:, :q1]))
    return jnp.concatenate(outs, axis=1).reshape(B, S, MLA_HEADS * MLA_V)


def ab_mixer(x, pos, w_in, gm_ln_g, gm_ln_b, gm_ws, gm_bs, q_norm_g, kv_norm_g,
             w_uq, w_uk, w_uv, w_o):
    h = x @ w_in
    u, v, c_q, c_kv, k_r = jnp.split(h, AB_SPLITS, axis=-1)
    y_a = gmlp_mix(jax.nn.gelu(u), jax.nn.gelu(v), gm_ln_g, gm_ln_b, gm_ws, gm_bs)
    y_b = mla_mix(c_q, c_kv, k_r, pos, q_norm_g, kv_norm_g, w_uq, w_uk, w_uv)
    return jnp.concatenate([y_a, y_b], axis=-1) @ w_o


def compress_kv(k, pos_emb, w1, w2):
    B, S, G, D = k.shape
    r = NSA_CMP_LEN // NSA_CMP_STRIDE
    sub = k.reshape(B, S // NSA_CMP_STRIDE, NSA_CMP_STRIDE, G, D)
    nc = S // NSA_CMP_STRIDE - r + 1
    blocks = jnp.concatenate([sub[:, j:j + nc] for j in range(r)], axis=2) + pos_emb[:, None, :]
    flat = blocks.transpose(0, 1, 3, 2, 4).reshape(B, nc, G, NSA_CMP_LEN * D)
    return jax.nn.gelu(flat @ w1) @ w2


def nsa_compressed(q, k_cmp, v_cmp):
    S, nc = q.shape[1], k_cmp.shape[1]
    s = jnp.einsum('btgid,bngd->bgitn', q, k_cmp).astype(jnp.float32) * NSA_DH ** -0.5
    end = jnp.arange(nc) * NSA_CMP_STRIDE + NSA_CMP_LEN - 1
    mask = jnp.arange(S)[:, None] >= end[None, :]
    p = jnp.where(mask, jax.nn.softmax(jnp.where(mask, s, NEG), axis=-1), 0.0)
    o = jnp.einsum('bgitn,bngd->btgid', p.astype(v_cmp.dtype), v_cmp)
    return o, p


def nsa_select_blocks(p_cmp):
    S, nc = p_cmp.shape[3], p_cmp.shape[4]
    nsel = S // NSA_SEL_LEN
    c0 = jnp.arange(nc) * NSA_CMP_STRIDE
    s0 = jnp.arange(nsel) * NSA_SEL_LEN
    cover = ((c0[:, None] < s0[None, :] + NSA_SEL_LEN)
             & (c0[:, None] + NSA_CMP_LEN > s0[None, :])).astype(jnp.float32)
    imp = jnp.einsum('bgitn,nj->bgtj', p_cmp, cover)
    jj = jnp.arange(nsel)[None, :]
    tb = (jnp.arange(S) // NSA_SEL_LEN)[:, None]
    forced = (jj == 0) | (jj == tb) | (jj == tb - 1)
    score = jnp.where(forced, NSA_FORCE, jnp.where(jj <= tb, imp, -NSA_FORCE))
    _, idx = lax.top_k(score, min(NSA_TOPK, nsel))
    return idx


def nsa_selected(q, k_s, v_s, sel_idx):
    B, S, G, HPG, D = q.shape
    K = sel_idx.shape[-1]
    nsel = S // NSA_SEL_LEN
    kb = k_s.reshape(B, nsel, NSA_SEL_LEN, G, D).transpose(0, 3, 1, 2, 4)
    vb = v_s.reshape(B, nsel, NSA_SEL_LEN, G, D).transpose(0, 3, 1, 2, 4)
    nqb = S // NSA_SEL_QBLOCK
    q_b = q.reshape(B, nqb, NSA_SEL_QBLOCK, G, HPG, D).transpose(1, 0, 2, 3, 4, 5)
    idx_b = sel_idx.reshape(B, G, nqb, NSA_SEL_QBLOCK, K).transpose(2, 0, 1, 3, 4)
    bi = jnp.arange(B)[:, None, None, None]
    gi = jnp.arange(G)[None, :, None, None]

    def one_block(args):
        i, qi, idx = args
        tq = i * NSA_SEL_QBLOCK + jnp.arange(NSA_SEL_QBLOCK)
        kg = kb[bi, gi, idx]
        vg = vb[bi, gi, idx]
        kpos = idx[..., None] * NSA_SEL_LEN + jnp.arange(NSA_SEL_LEN)
        mask = kpos <= tq[None, None, :, None, None]
        s = jnp.einsum('bqgid,bgqkld->bgiqkl', qi, kg).astype(jnp.float32) * NSA_DH ** -0.5
        s = jnp.where(mask[:, :, None], s, NEG)
        p = jax.nn.softmax(s.reshape(B, G, HPG, NSA_SEL_QBLOCK, K * NSA_SEL_LEN), axis=-1)
        p = p.reshape(s.shape).astype(vg.dtype)
        return jnp.einsum('bgiqkl,bgqkld->bqgid', p, vg)

    o = lax.map(one_block, (jnp.arange(nqb), q_b, idx_b))
    return o.transpose(1, 0, 2, 3, 4, 5).reshape(B, S, G, HPG, D)


def nsa_window(q, k_w, v_w):
    B, S, G, HPG, D = q.shape
    nb = S // Q_BLOCK
    span = NSA_WINDOW + Q_BLOCK
    kp = jnp.pad(k_w, ((0, 0), (NSA_WINDOW, 0), (0, 0), (0, 0)))
    vp = jnp.pad(v_w, ((0, 0), (NSA_WINDOW, 0), (0, 0), (0, 0)))
    q_b = q.reshape(B, nb, Q_BLOCK, G, HPG, D).transpose(1, 0, 2, 3, 4, 5)

    def one_block(args):
        i, qi = args
        start = i * Q_BLOCK
        kb = lax.dynamic_slice_in_dim(kp, start, span, axis=1)
        vb = lax.dynamic_slice_in_dim(vp, start, span, axis=1)
        tq = (start + jnp.arange(Q_BLOCK))[:, None]
        kpos = (start - NSA_WINDOW + jnp.arange(span))[None, :]
        mask = (kpos <= tq) & (kpos > tq - NSA_WINDOW) & (kpos >= 0)
        s = jnp.einsum('bqgid,bkgd->bgiqk', qi, kb).astype(jnp.float32) * NSA_DH ** -0.5
        p = jax.nn.softmax(jnp.where(mask, s, NEG), axis=-1).astype(vb.dtype)
        return jnp.einsum('bgiqk,bkgd->bqgid', p, vb)

    o = lax.map(one_block, (jnp.arange(nb), q_b))
    return o.transpose(1, 0, 2, 3, 4, 5).reshape(B, S, G, HPG, D)


def nsa_mixer(x, w_in, cmp_pos, w_ck1, w_ck2, w_cv1, w_cv2, gate_b, w_o):
    B, S, _ = x.shape
    h = x @ w_in
    q, kc, vc, ks, vs, kw, vw, g = jnp.split(h, NSA_SPLITS, axis=-1)
    q = q.reshape(B, S, NSA_GROUPS, NSA_HPG, NSA_DH)

    def kv(t):
        return t.reshape(B, S, NSA_GROUPS, NSA_DH)

    k_cmp = compress_kv(kv(kc), cmp_pos[0], w_ck1, w_ck2)
    v_cmp = compress_kv(kv(vc), cmp_pos[1], w_cv1, w_cv2)
    o_cmp, p_cmp = nsa_compressed(q, k_cmp, v_cmp)
    sel_idx = nsa_select_blocks(p_cmp)
    o_slc = nsa_selected(q, kv(ks), kv(vs), sel_idx)
    o_win = nsa_window(q, kv(kw), kv(vw))
    gates = jax.nn.sigmoid(g + gate_b).reshape(B, S, NSA_BRANCHES, NSA_GROUPS, NSA_HPG, 1)
    o = gates[:, :, 0] * o_cmp + gates[:, :, 1] * o_slc + gates[:, :, 2] * o_win
    return o.reshape(B, S, C_MIX) @ w_o


def hier_moe(x, w_rg, b_rg, w_re, b_re, w_gate, w_up, w_down):
    B, S, D = x.shape
    T = B * S
    xt = x.reshape(T, D)
    g_logits = (xt @ w_rg + b_rg).astype(jnp.float32)
    g_prob = jax.nn.softmax(g_logits, axis=-1)
    g_sel = jnp.argmax(g_logits, axis=-1)
    g_w = jnp.take_along_axis(g_prob, g_sel[:, None], axis=-1)
    e_logits = (xt @ w_re + b_re).astype(jnp.float32).reshape(T, MOE_GROUPS, MOE_EPG)
    e_sel = jnp.take_along_axis(e_logits, g_sel[:, None, None], axis=1)[:, 0]
    top_p, top_i = lax.top_k(jax.nn.softmax(e_sel, axis=-1), MOE_TOPK)
    top_p = top_p / top_p.sum(-1, keepdims=True)
    w_grp = (jax.nn.one_hot(top_i, MOE_EPG, dtype=jnp.float32) * top_p[..., None]).sum(1) * g_w
    y = jnp.zeros_like(xt)
    for gi in range(MOE_GROUPS):
        e0 = gi * MOE_EPG
        wg = jnp.where(g_sel[:, None] == gi, w_grp, 0.0).astype(x.dtype)
        hg = (jax.nn.silu(jnp.einsum('td,edf->tef', xt, w_gate[e0:e0 + MOE_EPG]))
              * jnp.einsum('td,edf->tef', xt, w_up[e0:e0 + MOE_EPG]))
        y = y + jnp.einsum('tef,efd->td', hg * wg[..., None], w_down[e0:e0 + MOE_EPG])
    return y.reshape(B, S, D)


def setup_inputs(seed: int = 0) -> dict:
    key = jax.random.key(seed)
    keys = iter(jax.random.split(key, 48))

    def nrm(shape, scale):
        return jax.random.normal(next(keys), shape, jnp.float32) * scale

    def gain(shape):
        return 1.0 + nrm(shape, 0.1)

    x = nrm((BATCH, SEQ, D_MODEL), 1.0)
    offset = jax.random.randint(next(keys), (BATCH, 1), 0, 4096, dtype=jnp.int32)
    positions = (offset + jnp.arange(SEQ, dtype=jnp.int32)[None, :]).astype(jnp.int32)
    return {
        'x': x,
        'positions': positions,
        'ab_w_in': nrm((N_EVEN, D_MODEL, AB_IN), D_MODEL ** -0.5),
        'ab_gm_ln_g': gain((N_EVEN, GM_WIDTH)),
        'ab_gm_ln_b': nrm((N_EVEN, GM_WIDTH), 0.02),
        'ab_gm_ws': nrm((N_EVEN, GM_GROUPS, GM_CHUNK, GM_CHUNK), GM_CHUNK ** -0.5),
        'ab_gm_bs': gain((N_EVEN, GM_GROUPS, GM_CHUNK)),
        'ab_mla_q_norm': gain((N_EVEN, MLA_Q_RANK)),
        'ab_mla_kv_norm': gain((N_EVEN, MLA_KV_RANK)),
        'ab_mla_w_uq': nrm((N_EVEN, MLA_Q_RANK, MLA_HEADS * (MLA_NOPE + MLA_ROPE)), MLA_Q_RANK ** -0.5),
        'ab_mla_w_uk': nrm((N_EVEN, MLA_KV_RANK, MLA_HEADS * MLA_NOPE), MLA_KV_RANK ** -0.5),
        'ab_mla_w_uv': nrm((N_EVEN, MLA_KV_RANK, MLA_HEADS * MLA_V), MLA_KV_RANK ** -0.5),
        'ab_w_o': nrm((N_EVEN, AB_MIX, D_MODEL), AB_MIX ** -0.5 * DN_BETA),
        'c_w_in': nrm((N_ODD, D_MODEL, C_IN), D_MODEL ** -0.5),
        'c_cmp_pos': nrm((N_ODD, 2, NSA_CMP_LEN, NSA_DH), 0.1),
        'c_w_ck1': nrm((N_ODD, NSA_CMP_LEN * NSA_DH, NSA_CMP_HIDDEN), (NSA_CMP_LEN * NSA_DH) ** -0.5),
        'c_w_ck2': nrm((N_ODD, NSA_CMP_HIDDEN, NSA_DH), NSA_CMP_HIDDEN ** -0.5),
        'c_w_cv1': nrm((N_ODD, NSA_CMP_LEN * NSA_DH, NSA_CMP_HIDDEN), (NSA_CMP_LEN * NSA_DH) ** -0.5),
        'c_w_cv2': nrm((N_ODD, NSA_CMP_HIDDEN, NSA_DH), NSA_CMP_HIDDEN ** -0.5),
        'c_gate_b': nrm((N_ODD, NSA_BRANCHES * NSA_HEADS), 0.1),
        'c_w_o': nrm((N_ODD, C_MIX, D_MODEL), C_MIX ** -0.5 * DN_BETA),
        'moe_w_rg': nrm((DEPTH, D_MODEL, MOE_GROUPS), D_MODEL ** -0.5),
        'moe_b_rg': nrm((DEPTH, MOE_GROUPS), 0.01),
        'moe_w_re': nrm((DEPTH, D_MODEL, MOE_EXPERTS), D_MODEL ** -0.5),
        'moe_b_re': nrm((DEPTH, MOE_EXPERTS), 0.01),
        'moe_w_gate': nrm((DEPTH, MOE_EXPERTS, D_MODEL, MOE_HIDDEN), D_MODEL ** -0.5),
        'moe_w_up': nrm((DEPTH, MOE_EXPERTS, D_MODEL, MOE_HIDDEN), D_MODEL ** -0.5),
        'moe_w_down': nrm((DEPTH, MOE_EXPERTS, MOE_HIDDEN, D_MODEL), MOE_HIDDEN ** -0.5 * DN_BETA),
        'ln1_g': gain((DEPTH, D_MODEL)),
        'ln1_b': nrm((DEPTH, D_MODEL), 0.02),
        'ln2_g': gain((DEPTH, D_MODEL)),
        'ln2_b': nrm((DEPTH, D_MODEL), 0.02),
    }


def reference(x, positions, ab_w_in, ab_gm_ln_g, ab_gm_ln_b, ab_gm_ws, ab_gm_bs,
              ab_mla_q_norm, ab_mla_kv_norm, ab_mla_w_uq, ab_mla_w_uk, ab_mla_w_uv, ab_w_o,
              c_w_in, c_cmp_pos, c_w_ck1, c_w_ck2, c_w_cv1, c_w_cv2, c_gate_b, c_w_o,
              moe_w_rg, moe_b_rg, moe_w_re, moe_b_re, moe_w_gate, moe_w_up, moe_w_down,
              ln1_g, ln1_b, ln2_g, ln2_b):
    for layer in range(DEPTH):
        j = layer // 2
        if layer % 2 == 0:
            mix = ab_mixer(x, positions, ab_w_in[j], ab_gm_ln_g[j], ab_gm_ln_b[j], ab_gm_ws[j],
                           ab_gm_bs[j], ab_mla_q_norm[j], ab_mla_kv_norm[j], ab_mla_w_uq[j],
                           ab_mla_w_uk[j], ab_mla_w_uv[j], ab_w_o[j])
        else:
            mix = nsa_mixer(x, c_w_in[j], c_cmp_pos[j], c_w_ck1[j], c_w_ck2[j], c_w_cv1[j],
                            c_w_cv2[j], c_gate_b[j], c_w_o[j])
        x = layer_norm(DN_ALPHA * x + mix, ln1_g[layer], ln1_b[layer])
        ffn = hier_moe(x, moe_w_rg[layer], moe_b_rg[layer], moe_w_re[layer], moe_b_re[layer],
                       moe_w_gate[layer], moe_w_up[layer], moe_w_down[layer])
        x = layer_norm(DN_ALPHA * x + ffn, ln2_g[layer], ln2_b[layer])
    return x
```

```python
import contextlib
import math
import numpy as np
import concourse.bass as bass
import concourse.mybir as mybir
from concourse.bass_utils import run_bass_kernel_spmd

F32 = mybir.dt.float32
BF16 = mybir.dt.bfloat16
I32 = mybir.dt.int32
AF = mybir.ActivationFunctionType
ALU = mybir.AluOpType
AX = mybir.AxisListType
NDS = 24
S = 2048
D = 1024
NT = 16
ALPHA = 4.0 ** 0.25
EPS = 1e-5


class Dep:
    __slots__ = ("w", "r", "excl")

    def __init__(self, excl=False):
        self.w = None
        self.r = {}
        self.excl = excl


class KB:
    def __init__(self, nc, stack):
        self.nc = nc
        self.stack = stack
        self.E = {"pe": nc.tensor, "act": nc.scalar, "dve": nc.vector,
                  "pool": nc.gpsimd, "sp": nc.sync}
        self.sem = {e: stack.enter_context(nc.semaphore(e + "_sem"))
                    for e in ("pe", "act", "dve", "pool")}
        self.cnt = {e: 0 for e in self.sem}
        self.known = {e: {} for e in self.E}
        self.dsem = [stack.enter_context(nc.semaphore("dq%d" % i)) for i in range(NDS)]
        self.dcnt = [0] * NDS
        self.dnext = 0
        self.dnextp = 0
        self.nins = 0

    def _wait(self, eng, evs):
        e = self.E[eng]
        need = {}
        for ev in evs:
            if ev is None:
                continue
            s, v = ev
            if eng == "pe" and s is self.sem["pe"]:
                continue
            if self.known[eng].get(id(s), 0) >= v:
                continue
            if eng in self.sem and s is self.sem[eng]:
                assert v <= self.cnt[eng], "same-engine dep on unsignaled op (%s)" % eng
            if need.get(id(s), (None, 0))[1] < v:
                need[id(s)] = (s, v)
        for s, v in need.values():
            e.wait_ge(s, v)
            self.known[eng][id(s)] = v

    def _evs(self, reads, writes, eng=None):
        evs = []
        own = self.sem.get(eng)
        for d in reads:
            evs.append(d.w)
            if d.excl:
                evs.extend(ev for ev in d.r.values() if ev[0] is not own)
        for d in writes:
            evs.append(d.w)
            evs.extend(d.r.values())
        return evs

    @staticmethod
    def _upd(ev, reads, writes):
        s, v = ev
        for d in reads:
            o = d.r.get(id(s))
            if o is None or o[1] < v:
                d.r[id(s)] = ev
        for d in writes:
            d.w = ev
            d.r = {}

    def op(self, eng, fn, reads=(), writes=(), signal=True):
        self._wait(eng, self._evs(reads, writes, eng))
        ins = fn(self.E[eng])
        self.nins += 1
        if signal:
            self.cnt[eng] += 1
            ins.then_inc(self.sem[eng], 1)
            ev = (self.sem[eng], self.cnt[eng])
        else:
            ev = (self.sem[eng], self.cnt[eng] + 1)
        self._upd(ev, reads, writes)
        return ev

    def dma(self, q, out, in_, reads=(), writes=(), **kw):
        if q == "pool":
            j = 16 + self.dnextp
            self.dnextp = (self.dnextp + 1) % (NDS - 16)
        else:
            j = self.dnext
            self.dnext = (j + 1) % 16
        evs = self._evs(reads, writes)
        if self.dcnt[j] > 0:
            evs.append((self.dsem[j], self.dcnt[j]))
        self._wait(q, evs)
        ins = self.E[q].dma_start(out=out, in_=in_, **kw)
        self.nins += 1
        self.dcnt[j] += 16
        ins.then_inc(self.dsem[j], 16)
        ev = (self.dsem[j], self.dcnt[j])
        self._upd(ev, reads, writes)
        return ev

    def barrier(self):
        evs = [(self.sem[e], self.cnt[e]) for e in self.sem if self.cnt[e] > 0]
        evs += [(self.dsem[j], self.dcnt[j]) for j in range(NDS) if self.dcnt[j] > 0]
        for eng in self.E:
            self._wait(eng, evs)

    def wait_all(self, eng, deps):
        evs = []
        for d in deps:
            evs.append(d.w)
            evs.extend(d.r.values())
        self._wait(eng, evs)

    def sb(self, name, shape, dt, stack=None):
        self.nsb = getattr(self, "nsb", 0) + 1
        return (stack or self.stack).enter_context(self.nc.sbuf_tensor("%s_s%d" % (name, self.nsb), list(shape), dt))


class Ctx:
    pass


def build(nseq=2, stop=99):
    nc = bass.Bass("TRN2", target_bir_lowering=False)
    di = {}

    def inp(name, shape, dt=F32):
        di[name] = nc.dram_tensor(name, list(shape), dt, kind="ExternalInput").ap()
        return di[name]

    x_d = inp("x", [nseq, S, D])
    posT_d = inp("posT", [128, nseq * NT], I32)
    inp("ab_w_in", [D, 1440]); inp("gm_ln_g", [1, 512]); inp("gm_ln_b", [1, 512])
    inp("gm_wsT", [4, 128, 128]); inp("gm_bs", [1, 512])
    inp("qg2", [128, 2]); inp("kvg", [128, 1])
    inp("w_uq", [256, 768]); inp("w_uk", [128, 512]); inp("w_uv", [128, 512]); inp("ab_w_o", [D, D])
    inp("c_w_in", [D, 1840]); inp("cmp_posT", [2, 128, 16])
    inp("c_w_ck1", [2048, 256]); inp("c_w_ck2", [256, 64]); inp("c_w_cv1", [2048, 256]); inp("c_w_cv2", [256, 64])
    inp("c_gate_b", [1, 48]); inp("c_w_o", [D, D])
    inp("moe_w_rg", [2, D, 4]); inp("moe_b_rg", [2, 4]); inp("moe_w_re", [2, D, 32]); inp("moe_b_re", [2, 32])
    inp("moe_w_gate", [2, 32, D, 256]); inp("moe_w_up", [2, 32, D, 256]); inp("moe_w_down", [2, 32, 256, D])
    inp("ln1_g", [2, D]); inp("ln1_b", [2, D]); inp("ln2_g", [2, D]); inp("ln2_b", [2, D])
    inp("ident", [128, 128]); inp("tri", [128, 128]); inp("tri2", [128, 128]); inp("freq", [128, 16])
    inp("selm", [32, 32 * 128]); inp("cmask", [128, S]); inp("cover", [128, 32])
    inp("cand", [128, NT * 32]); inp("sbias", [128, NT * 32]); inp("e2", [32, NT * 128])
    out_d = nc.dram_tensor("out", [nseq, S, D], F32, kind="ExternalOutput").ap()
    xs_d = nc.dram_tensor("xscr", [nseq, S, D], F32, kind="Internal").ap()

    with contextlib.ExitStack() as st:
        k = KB(nc, st)
        c = Ctx()
        c.nc, c.k, c.di, c.nseq = nc, k, di, nseq
        c.x_d, c.out_d, c.xs_d, c.posT_d = x_d, out_d, xs_d, posT_d
        c.stop = stop
        import os
        c.dbg = os.environ.get("KDBG", "")
        c.ps = [st.enter_context(nc.psum_tensor("ps%d" % i, [128, 512], F32)) for i in range(8)]
        c.dps = [Dep(excl=True) for _ in range(8)]
        c.rr = 0
        c.XT = k.sb("XT", [128, 8, S], BF16); c.dXT = [Dep() for _ in range(NT)]
        c.ident = k.sb("ident", [128, 128], F32); c.identb = k.sb("identb", [128, 128], BF16)
        c.tri = k.sb("tri", [128, 128], BF16); c.tri2 = k.sb("tri2", [128, 128], BF16)
        c.dconst = Dep()
        tmp = k.sb("ctmp", [128, 256], F32)
        k.dma("sp", c.ident[:], di["ident"][:, :], writes=[c.dconst])
        k.dma("sp", tmp[:, 0:128], di["tri"][:, :], writes=[c.dconst])
        k.dma("sp", tmp[:, 128:256], di["tri2"][:, :], writes=[c.dconst])
        k.op("dve", lambda e: e.tensor_copy(out=c.identb[:], in_=c.ident[:]), reads=[c.dconst], writes=[c.dconst])
        k.op("dve", lambda e: e.tensor_copy(out=c.tri[:], in_=tmp[:, 0:128]), reads=[c.dconst], writes=[c.dconst])
        k.op("dve", lambda e: e.tensor_copy(out=c.tri2[:], in_=tmp[:, 128:256]), reads=[c.dconst], writes=[c.dconst])
        c.ntri = k.sb("ntri", [128, 128], BF16); c.ntri2 = k.sb("ntri2", [128, 128], BF16)
        k.op("dve", lambda e: e.tensor_scalar(out=c.ntri[:], in0=tmp[:, 128:256], scalar1=-30000.0, scalar2=None, op0=ALU.mult),
             reads=[c.dconst], writes=[c.dconst])
        k.op("dve", lambda e: e.tensor_scalar(out=c.ntri2[:], in0=tmp[:, 0:128], scalar1=-30000.0, scalar2=None, op0=ALU.mult),
             reads=[c.dconst], writes=[c.dconst])
        c.outdeps = []
        c.dxs = [[Dep() for _ in range(NT)] for _ in range(nseq)]
        c.WG = k.sb("WG", [128, NT, 32], F32)
        c.dWG = Dep()
        c.epsb = k.sb("epsb", [128, 4], F32)
        k.op("pool", lambda e: e.memset(c.epsb[:, 0:1], EPS), writes=[c.dconst])
        k.op("pool", lambda e: e.memset(c.epsb[:, 1:2], -3.14159), writes=[c.dconst])
        k.op("pool", lambda e: e.memset(c.epsb[:, 2:4], -0.5), writes=[c.dconst])
        for s in range(nseq):
            layer0(c, s)
            if c.dbg:
                break
            with contextlib.ExitStack() as wst:
                W = moe_prefetch(c, 0, wst) if stop > 1 else None
                mixer_out(c, s, 0, "ab_w_o", c.x_d)
                if stop > 1:
                    moe(c, s, 0, (stop == 2), W)
            if stop <= 2:
                continue
            layer1(c, s)
            with contextlib.ExitStack() as wst:
                W = moe_prefetch(c, 1, wst) if stop > 3 else None
                mixer_out(c, s, 1, "c_w_o", c.xs_d)
                if stop > 3:
                    moe(c, s, 1, True, W)
        k.wait_all("sp", c.outdeps)
        c.nins = k.nins
    return nc, c


def bank(c, i):
    return c.ps[i], c.dps[i]


def transpose_to_XT(c, src, dsrc, i, extra32=None, dextra=None, banks=(0, 1)):
    k = c.k
    for half in range(2):
        ps, dp = bank(c, banks[half])
        for j in range(4):
            kc = half * 4 + j
            k.op("pe", lambda e: e.transpose(out=ps[:, j * 128:(j + 1) * 128], in_=src[:, kc * 128:(kc + 1) * 128],
                                             identity=c.ident[:]),
                 reads=[dsrc, c.dconst], writes=[dp], signal=(j == 3))
        psv = ps[:].rearrange("p (j t) -> p j t", j=4)
        k.op("act" if half == 0 else "dve",
             (lambda e: e.copy(out=c.XT[:, half * 4:half * 4 + 4, i * 128:(i + 1) * 128], in_=psv)) if half == 0 else
             (lambda e: e.tensor_copy(out=c.XT[:, half * 4:half * 4 + 4, i * 128:(i + 1) * 128], in_=psv)),
             reads=[dp], writes=[c.dXT[i]])
        if extra32 is not None:
            k.op("dve" if half == 0 else "act",
                 (lambda e: e.tensor_copy(out=extra32[:, half * 4:half * 4 + 4, :], in_=psv)) if half == 0 else
                 (lambda e: e.copy(out=extra32[:, half * 4:half * 4 + 4, :], in_=psv)),
                 reads=[dp], writes=[dextra])


def layer_norm_tile(c, R, dR, G, B, dGB, out, dout, st32, dst, par=0):
    k = c.k
    stats, mv, rstd = st32[par % len(st32)]
    dst = dst[par % len(dst)]
    for h in range(2):
        k.op("dve", lambda e: e.bn_stats(out=stats[:, h * 6:(h + 1) * 6], in_=R[:, h * 512:(h + 1) * 512]),
             reads=[dR], writes=[dst])
    k.op("dve", lambda e: e.bn_aggr(out=mv[:, 0:2], in_=stats[:, 0:12]), reads=[dst], writes=[dst])
    k.op("pool", lambda e: e.tensor_scalar(out=rstd[:, 0:1], in0=mv[:, 1:2], scalar1=EPS, scalar2=None, op0=ALU.add), reads=[dst], writes=[dst])
    k.op("pool", lambda e: e.tensor_tensor(out=rstd[:, 1:2], in0=rstd[:, 0:1], in1=c.epsb[:, 2:3], op=ALU.pow), reads=[dst, c.dconst], writes=[dst])
    k.op("dve", lambda e: e.tensor_scalar(out=R[:], in0=R[:], scalar1=mv[:, 0:1], scalar2=rstd[:, 1:2],
                                          op0=ALU.subtract, op1=ALU.mult), reads=[dR, dst], writes=[dR])
    k.op("dve", lambda e: e.tensor_tensor(out=R[:], in0=R[:], in1=G[:], op=ALU.mult), reads=[dR, dGB], writes=[dR])
    k.op("pool", lambda e: e.tensor_tensor(out=out[:, 0:384], in0=R[:, 0:384], in1=B[:, 0:384], op=ALU.add), reads=[dR, dGB], writes=[dout])
    k.op("dve", lambda e: e.tensor_tensor(out=out[:, 384:1024], in0=R[:, 384:1024], in1=B[:, 384:1024], op=ALU.add), reads=[dR, dGB], writes=[dout])


def mk_st32(c, ph, n=2):
    k = c.k
    return ([(k.sb("stats", [128, 12], F32, ph), k.sb("mv", [128, 2], F32, ph), k.sb("rstd", [128, 4], F32, ph)) for _ in range(n)],
            [Dep() for _ in range(n)])


def load_bf16(c, dst, src, dep):
    c.k.dma("pool", dst, src, writes=[dep])


def attention(c, ph, nheads, hg_size, qk_fn, kt_range_fn, ts_range_fn, mask_fn, v_fn, nv, fin_fn, tb, PT, dPT,
              acc_banks, s_banks, LA=4):
    k = c.k
    kts = kt_range_fn(tb)
    items = []
    for hg in range(nheads // hg_size):
        for kt in kts:
            for hi_ in range(hg_size):
                items.append((hg, kt, hi_, hg * hg_size + hi_))
    st = {}

    def front(n):
        hg, kt, hi_, h = items[n]
        lo, hi = ts_range_fn(tb, kt)
        c0, c1 = lo * 128, (hi + 1) * 128
        ps, dp = bank(c, s_banks[n % len(s_banks)])
        pt, dpt = PT[n % len(PT)], dPT[n % len(PT)]
        lhsT, rhs, rds = qk_fn(h, kt, tb * 512 + c0, tb * 512 + c1)
        masks = mask_fn(h, kt, lo, hi)
        k.op("pe", lambda e: e.matmul(ps[:, c0:c1], lhsT=lhsT, rhs=rhs, start=True, stop=(len(masks) == 0)), reads=rds, writes=[dp],
             signal=(len(masks) == 0))
        for mi_, (m0, m1, ml, mr_, mrd) in enumerate(masks):
            last = (mi_ == len(masks) - 1)
            k.op("pe", lambda e: e.matmul(ps[:, m0:m1], lhsT=ml, rhs=mr_, start=False, stop=last), reads=mrd, writes=[dp], signal=last)
        k.op("act", lambda e: e.activation(out=pt[:, c0:c1], in_=ps[:, c0:c1], func=AF.Exp), reads=[dp], writes=[dpt])
        st[n] = (pt, dpt, lo, hi)

    def finish(hg):
        for ts in range(4):
            pa, dpa = bank(c, acc_banks[ts])
            fin_fn(hg, ts, pa[:, 0:hg_size * nv].rearrange("p (h v) -> p h v", h=hg_size), dpa)

    def back(n):
        hg, kt, hi_, h = items[n]
        if n == 0 or items[n - 1][0] != hg:
            if n > 0:
                finish(items[n - 1][0])
            for ts in range(4):
                pa, dpa = bank(c, acc_banks[ts])
                k.op("dve", lambda e: e.memset(pa[:, 0:hg_size * nv], 0.0), writes=[dpa])
        pt, dpt, lo, hi = st.pop(n)
        vr, vrd = v_fn(h, kt)
        for ts in range(lo, hi + 1):
            pa, dpa = bank(c, acc_banks[ts])
            k.op("pe", lambda e: e.matmul(pa[:, hi_ * nv:(hi_ + 1) * nv], lhsT=pt[:, ts * 128:(ts + 1) * 128],
                                          rhs=vr, start=False, stop=False, skip_group_check=True),
                 reads=[dpt] + vrd, writes=[dpa], signal=(ts == hi))

    for n in range(min(LA, len(items))):
        front(n)
    for n in range(len(items)):
        back(n)
        if n + LA < len(items):
            front(n + LA)
    finish(items[-1][0])


def layer0(c, s):
    k, nc, di = c.k, c.nc, c.di
    with contextlib.ExitStack() as ph:
      p1 = ph.enter_context(contextlib.ExitStack())
      if True:
        XT, dXT = c.XT, c.dXT
        dW = Dep()
        KT = k.sb("KT", [128, 8, S], BF16, ph); dKT = [Dep() for _ in range(NT)]
        VA = k.sb("VA", [128, NT, 8, 65], BF16, ph); dVA = [Dep() for _ in range(NT)]
        CQT = k.sb("CQT", [128, 2, S], BF16, ph); dCQ = [Dep() for _ in range(NT)]
        RQ = k.sb("RQ", [128, NT], F32, ph)
        COS = k.sb("COS", [128, NT, 16], F32, ph)
        SIN = k.sb("SIN", [128, NT, 16], F32, ph)
        wuq = k.sb("wuq", [128, 2, 768], BF16, ph)
        xin = [k.sb("xin%d" % j, [128, D], F32, p1) for j in range(2)]
        dxin = [Dep(), Dep()]
        for i in range(2):
            k.dma("sp", xin[i][:], c.x_d[s, i * 128:(i + 1) * 128, :], writes=[dxin[i]])
        Win = k.sb("Win", [128, 8, 1440], BF16, p1)
        load_bf16(c, Win[:], di["ab_w_in"].rearrange("(kc p) n -> p kc n", p=128), dW)
        wuq32 = k.sb("wuq32", [128, 2, 768], F32, p1)
        wkv32 = k.sb("wkv32", [128, 1024], F32, p1)
        wkv = k.sb("wkv", [128, 1024], BF16, p1)
        qg = k.sb("qg", [128, 4], F32, p1)
        k.dma("sp", wuq32[:], di["w_uq"].rearrange("(kc p) n -> p kc n", p=128), writes=[dW])
        k.dma("sp", wkv32[:, 0:512], di["w_uk"][:, :], writes=[dW])
        k.dma("sp", wkv32[:, 512:1024], di["w_uv"][:, :], writes=[dW])
        k.dma("sp", qg[:, 0:2], di["qg2"][:, :], writes=[dW])
        k.dma("sp", qg[:, 2:3], di["kvg"][:, :], writes=[dW])
        for cc in range(2):
            k.op("dve", lambda e: e.tensor_scalar(out=wuq[:, cc, :], in0=wuq32[:, cc, :], scalar1=qg[:, cc:cc + 1],
                                                  scalar2=None, op0=ALU.mult), reads=[dW], writes=[dW])
        k.op("dve", lambda e: e.tensor_scalar(out=wkv[:], in0=wkv32[:], scalar1=qg[:, 2:3], scalar2=None, op0=ALU.mult),
             reads=[dW], writes=[dW])
        wsT32 = k.sb("wsT32", [128, 4, 128], F32, p1)
        wsT = k.sb("wsT", [128, 4, 128], BF16, p1)
        k.dma("sp", wsT32[:], di["gm_wsT"].rearrange("g s t -> s g t"), writes=[dW])
        tri32 = k.sb("tri32", [128, 128], F32, p1)
        k.dma("sp", tri32[:], di["tri"][:, :], writes=[dW])
        k.op("dve", lambda e: e.tensor_tensor(out=wsT[:], in0=wsT32[:], in1=tri32[:].unsqueeze(1).to_broadcast([128, 4, 128]),
                                              op=ALU.mult), reads=[dW], writes=[dW])
        bsB = k.sb("bsB", [128, 512], F32, p1)
        gmG = k.sb("gmG", [128, 512], F32, p1)
        gmB = k.sb("gmB", [128, 512], F32, p1)
        k.dma("sp", bsB[:], di["gm_bs"][0:1, :].partition_broadcast(128), writes=[dW])
        k.dma("sp", gmG[:], di["gm_ln_g"][0:1, :].partition_broadcast(128), writes=[dW])
        k.dma("sp", gmB[:], di["gm_ln_b"][0:1, :].partition_broadcast(128), writes=[dW])
        freq = k.sb("freq", [128, 16], F32, p1)
        k.dma("sp", freq[:], di["freq"][:, :], writes=[dW])
        posi = k.sb("posi", [128, NT], I32, p1)
        posf = k.sb("posf", [128, NT], F32, p1)
        k.dma("sp", posi[:], c.posT_d[:, s * NT:(s + 1) * NT], writes=[dW])
        k.op("dve", lambda e: e.tensor_copy(out=posf[:], in_=posi[:]), reads=[dW], writes=[dW])
        ang = k.sb("ang", [128, NT, 16], F32, p1)
        angi = k.sb("angi", [128, NT, 16], I32, p1)
        angf = k.sb("angf", [128, NT, 16], F32, p1)
        dR = Dep()
        for (tab, off) in ((SIN, 0.5), (COS, 0.75)):
            k.op("dve", lambda e: e.tensor_tensor(out=ang[:], in0=posf[:].unsqueeze(2).to_broadcast([128, NT, 16]),
                                                  in1=freq[:].unsqueeze(1).to_broadcast([128, NT, 16]), op=ALU.mult),
                 reads=[dW, dR], writes=[dR])
            k.op("dve", lambda e: e.tensor_scalar(out=ang[:], in0=ang[:], scalar1=off, scalar2=None, op0=ALU.add),
                 reads=[dR], writes=[dR])
            k.op("dve", lambda e: e.tensor_copy(out=angi[:], in_=ang[:]), reads=[dR], writes=[dR])
            k.op("dve", lambda e: e.tensor_copy(out=angf[:], in_=angi[:]), reads=[dR], writes=[dR])
            k.op("dve", lambda e: e.tensor_tensor(out=ang[:], in0=ang[:], in1=angf[:], op=ALU.subtract), reads=[dR], writes=[dR])
            k.op("dve", lambda e: e.tensor_scalar(out=angf[:], in0=ang[:], scalar1=0.0, scalar2=None, op0=ALU.is_lt),
                 reads=[dR], writes=[dR])
            k.op("dve", lambda e: e.tensor_tensor(out=ang[:], in0=ang[:], in1=angf[:], op=ALU.add), reads=[dR], writes=[dR])
            k.op("act", lambda e: e.activation(out=tab[:], in_=ang[:], func=AF.Sin, bias=c.epsb[:, 1:2], scale=6.28318),
                 reads=[dR, c.dconst], writes=[dR])
        k.op("pool", lambda e: e.memset(VA[:], 1.0), writes=dVA)
        for i in range(NT):
            b = i % 2
            if i >= 2:
                k.dma("sp", xin[b][:], c.x_d[s, i * 128:(i + 1) * 128, :], writes=[dxin[b]])
            transpose_to_XT(c, xin[b], dxin[b], i, banks=(0 + 2 * b, 1 + 2 * b))
        if c.dbg == "p0":
            dbg_exit(c, s); p1.close(); return
        gv = k.sb("gv", [128, 512], F32, p1); dgv = Dep()
        vn = k.sb("vn", [128, 512], BF16, p1); dvn = Dep()
        gu = k.sb("gu", [128, 512], F32, p1); dgu = Dep()
        stt = (k.sb("stats", [128, 12], F32, p1), k.sb("mv", [128, 2], F32, p1), k.sb("rstd", [128, 4], F32, p1))
        dst = Dep()
        sq = k.sb("sq", [128, 512], F32, p1); dsq = Dep()
        ssq = k.sb("ssq", [128, 4], F32, p1); dss = Dep()
        ckvT = k.sb("ckvT", [128, 128], BF16, p1); dck = Dep()
        Kt = k.sb("Kt", [128, 8, 96], BF16, p1); dKt = Dep()
        kr = k.sb("kr", [128, 64], F32, p1); dkr = Dep()
        tmpS = k.sb("tmpS", [128, 512], F32, p1); dtS = Dep()
        for i in range(NT):
            tok = slice(i * 128, (i + 1) * 128)
            pv, dpv = bank(c, 0); pc, dpc = bank(c, 1); pu, dpu = bank(c, 2); pct, dpct = bank(c, 3)
            for kc in range(8):
                k.op("pe", lambda e: e.matmul(pv[:, 0:512], lhsT=XT[:, kc, tok], rhs=Win[:, kc, 512:1024], start=(kc == 0), stop=(kc == 7)),
                     reads=[dXT[i], dW], writes=[dpv], signal=(kc == 7))
            for kc in range(8):
                k.op("pe", lambda e: e.matmul(pc[:, 0:416], lhsT=XT[:, kc, tok], rhs=Win[:, kc, 1024:1440], start=(kc == 0), stop=(kc == 7)),
                     reads=[dXT[i], dW], writes=[dpc], signal=(kc == 7))
            for cc in range(4):
                for kc in range(8):
                    k.op("pe", lambda e: e.matmul(pu[:, cc * 128:(cc + 1) * 128], lhsT=Win[:, kc, cc * 128:(cc + 1) * 128], rhs=XT[:, kc, tok],
                                                  start=(kc == 0), stop=(kc == 7)),
                         reads=[dXT[i], dW], writes=[dpu], signal=(kc == 7 and cc == 3))
            for cc in range(3):
                for kc in range(8):
                    k.op("pe", lambda e: e.matmul(pct[:, cc * 128:(cc + 1) * 128], lhsT=Win[:, kc, 1024 + cc * 128:1024 + (cc + 1) * 128],
                                                  rhs=XT[:, kc, tok], start=(kc == 0), stop=(kc == 7)),
                         reads=[dXT[i], dW], writes=[dpct], signal=(kc == 7 and cc == 2))
            k.op("act", lambda e: e.activation(out=gv[:], in_=pv[:, 0:512], func=AF.Gelu_apprx_tanh), reads=[dpv], writes=[dgv])
            k.op("dve", lambda e: e.bn_stats(out=stt[0][:, 0:6], in_=gv[:]), reads=[dgv], writes=[dst])
            k.op("dve", lambda e: e.bn_aggr(out=stt[1][:, 0:2], in_=stt[0][:, 0:6]), reads=[dst], writes=[dst])
            k.op("pool", lambda e: e.tensor_scalar(out=stt[2][:, 0:1], in0=stt[1][:, 1:2], scalar1=EPS, scalar2=None, op0=ALU.add), reads=[dst], writes=[dst])
            k.op("pool", lambda e: e.tensor_tensor(out=stt[2][:, 1:2], in0=stt[2][:, 0:1], in1=c.epsb[:, 2:3], op=ALU.pow), reads=[dst, c.dconst], writes=[dst])
            k.op("dve", lambda e: e.tensor_scalar(out=gv[:], in0=gv[:], scalar1=stt[1][:, 0:1], scalar2=stt[2][:, 1:2],
                                                  op0=ALU.subtract, op1=ALU.mult), reads=[dgv, dst], writes=[dgv])
            k.op("pool", lambda e: e.tensor_tensor(out=gv[:], in0=gv[:], in1=gmG[:], op=ALU.mult), reads=[dgv, dW], writes=[dgv])
            k.op("pool", lambda e: e.tensor_tensor(out=vn[:], in0=gv[:], in1=gmB[:], op=ALU.add), reads=[dgv, dW], writes=[dvn])
            k.op("act", lambda e: e.activation(out=gu[:], in_=pu[:, 0:512], func=AF.Gelu_apprx_tanh), reads=[dpu], writes=[dgu])
            ps_, dps_ = bank(c, 4)
            for g in range(4):
                k.op("pe", lambda e: e.matmul(ps_[:, g * 128:(g + 1) * 128], lhsT=vn[:, g * 128:(g + 1) * 128], rhs=wsT[:, g, :],
                                              start=True, stop=True), reads=[dvn, dW], writes=[dps_], signal=(g == 3))
            k.op("dve", lambda e: e.tensor_tensor(out=tmpS[:], in0=ps_[:, 0:512], in1=bsB[:], op=ALU.add), reads=[dps_, dW], writes=[dtS])
            k.op("pool", lambda e: e.memset(ssq[:, 0:2], 0.0), writes=[dss])
            k.op("act", lambda e: e.activation(out=sq[:, 0:256], in_=pc[:, 0:256], func=AF.Square, accum_out=ssq[:, 0:1]),
                 reads=[dpc], writes=[dsq, dss])
            k.op("act", lambda e: e.activation(out=sq[:, 256:384], in_=pc[:, 256:384], func=AF.Square, accum_out=ssq[:, 1:2]),
                 reads=[dpc], writes=[dsq, dss])
            k.op("pool", lambda e: e.tensor_scalar(out=ssq[:, 2:3], in0=ssq[:, 0:1], scalar1=1.0 / 256, scalar2=EPS, op0=ALU.mult, op1=ALU.add),
                 reads=[dss], writes=[dss])
            k.op("pool", lambda e: e.tensor_scalar(out=ssq[:, 3:4], in0=ssq[:, 1:2], scalar1=1.0 / 128, scalar2=EPS, op0=ALU.mult, op1=ALU.add),
                 reads=[dss], writes=[dss])
            k.op("pool", lambda e: e.tensor_tensor(out=ssq[:, 0:2], in0=ssq[:, 2:4], in1=c.epsb[:, 2:4], op=ALU.pow), reads=[dss, c.dconst], writes=[dss])
            k.op("dve", lambda e: e.tensor_scalar(out=RQ[:, i:i + 1], in0=ssq[:, 0:1], scalar1=96.0 ** -0.5, scalar2=None, op0=ALU.mult),
                 reads=[dss], writes=[dCQ[i]])
            k.op("act", lambda e: e.copy(out=kr[:, 0:32], in_=pc[:, 384:416]), reads=[dpc], writes=[dkr])
            rope(c, kr[:, 0:32].rearrange("p (o r) -> p o r", o=1), Kt[:, 0:1, 64:96], COS[:, i:i + 1, :], SIN[:, i:i + 1, :],
                 kr[:, 32:64].rearrange("p (o r) -> p o r", o=1), [dkr, dR], dkr, dKt, 1)
            for h in range(1, 8):
                k.op("pool", lambda e: e.tensor_copy(out=Kt[:, h, 64:96], in_=Kt[:, 0, 64:96]), reads=[dKt], writes=[dKt])
            k.op("dve", lambda e: e.tensor_tensor(out=XT[:, 0:4, tok], in0=tmpS[:].rearrange("p (g t) -> p g t", g=4),
                                                  in1=gu[:].rearrange("p (g t) -> p g t", g=4), op=ALU.mult),
                 reads=[dtS, dgu], writes=[dXT[i]])
            k.op("act", lambda e: e.copy(out=CQT[:, :, tok], in_=pct[:, 0:256].rearrange("p (c t) -> p c t", c=2)),
                 reads=[dpct], writes=[dCQ[i]])
            k.op("act", lambda e: e.copy(out=ckvT[:], in_=pct[:, 256:384]), reads=[dpct], writes=[dck])
            pk, dpk = bank(c, 5); pvv, dpvv = bank(c, 6)
            k.op("pe", lambda e: e.matmul(pk[:, 0:512], lhsT=ckvT[:], rhs=wkv[:, 0:512], start=True, stop=True), reads=[dck, dW], writes=[dpk])
            k.op("pe", lambda e: e.matmul(pvv[:, 0:512], lhsT=ckvT[:], rhs=wkv[:, 512:1024], start=True, stop=True), reads=[dck, dW], writes=[dpvv])
            k.op("dve", lambda e: e.tensor_scalar(out=Kt[:, :, 0:64], in0=pk[:, 0:512].rearrange("p (h d) -> p h d", h=8),
                                                  scalar1=ssq[:, 1:2], scalar2=None, op0=ALU.mult), reads=[dpk, dss], writes=[dKt])
            k.op("act", lambda e: e.activation(out=VA[:, i, :, 0:64], in_=pvv[:, 0:512].rearrange("p (h d) -> p h d", h=8),
                                               func=AF.Copy, scale=ssq[:, 1:2]), reads=[dpvv, dss], writes=[dVA[i]])
            ptr, dptr = bank(c, 7)
            ptrb = ptr[:].bitcast(BF16)
            for h in range(8):
                k.op("pe", lambda e: e.transpose(out=ptrb[0:96, h * 128:(h + 1) * 128], in_=Kt[:, h, :], identity=c.identb[:]),
                     reads=[dKt, c.dconst], writes=[dptr], signal=(h == 7))
            k.op("act", lambda e: e.copy(out=KT[0:96, :, tok], in_=ptrb[0:96, :].rearrange("p (h t) -> p h t", h=8)),
                 reads=[dptr], writes=[dKT[i]])
        if c.dbg == "p1":
            dbg_exit(c, s); p1.close(); return
        k.barrier()
        p1.close()
        QT = [k.sb("QT%d" % j, [128, 8, 512], BF16, ph) for j in range(2)]
        dQT = [Dep(), Dep()]
        Qt = k.sb("Qt", [128, 8, 96], BF16, ph); dQt = Dep()
        qr = k.sb("qr", [128, 8, 32], F32, ph); dqr = Dep()
        qtmp = k.sb("qtmp", [128, 8, 32], F32, ph)
        PT = [k.sb("PT%d" % j, [128, 512], BF16, ph) for j in range(6)]
        dPT = [Dep() for _ in range(6)]
        Ot = k.sb("Ot", [128, 4, 512], BF16, ph); dOt = [Dep() for _ in range(4)]
        rden = k.sb("rden", [128, 8], F32, ph); drd = Dep()
        if c.dbg == "p2z":
            dbg_exit(c, s); return
        def qprep(tb):
            qb = tb % 2
            for ts in range(4):
                i = tb * 4 + ts
                tok = slice(i * 128, (i + 1) * 128)
                pq = [bank(c, 4), bank(c, 5)]
                for hf in range(2):
                    for cc in range(2):
                        k.op("pe", lambda e: e.matmul(pq[hf][0][:, 0:384], lhsT=CQT[:, cc, tok], rhs=wuq[:, cc, hf * 384:(hf + 1) * 384],
                                                      start=(cc == 0), stop=(cc == 1)), reads=[dCQ[i], dW], writes=[pq[hf][1]],
                             signal=(cc == 1))
                for hf in range(2):
                    pqv = pq[hf][0][:, 0:384].rearrange("p (h d) -> p h d", h=4)
                    k.op("dve", lambda e: e.tensor_scalar(out=Qt[:, hf * 4:hf * 4 + 4, 0:64], in0=pqv[:, :, 0:64], scalar1=RQ[:, i:i + 1],
                                                          scalar2=None, op0=ALU.mult), reads=[pq[hf][1], dCQ[i]], writes=[dQt])
                    k.op("dve", lambda e: e.tensor_scalar(out=qr[:, hf * 4:hf * 4 + 4, :], in0=pqv[:, :, 64:96], scalar1=RQ[:, i:i + 1],
                                                          scalar2=None, op0=ALU.mult), reads=[pq[hf][1], dCQ[i]], writes=[dqr])
                rope(c, qr[:], Qt[:, :, 64:96], COS[:, i:i + 1, :], SIN[:, i:i + 1, :], qtmp[:], [dqr, dR], dqr, dQt, 8)
                ptr, dptr = bank(c, 6 + (ts % 2))
                ptrb = ptr[:].bitcast(BF16)
                for h in range(8):
                    k.op("pe", lambda e: e.transpose(out=ptrb[0:96, h * 128:(h + 1) * 128], in_=Qt[:, h, :], identity=c.identb[:]),
                         reads=[dQt, c.dconst], writes=[dptr], signal=(h == 7))
                k.op("act", lambda e: e.copy(out=QT[qb][0:96, :, ts * 128:(ts + 1) * 128],
                                             in_=ptrb[0:96, :].rearrange("p (h t) -> p h t", h=8)), reads=[dptr], writes=[dQT[qb]])

        qprep(0)
        for tb in range(4):
            qb = tb % 2
            if tb + 1 < 4:
                qprep(tb + 1)

            def qk_fn(h, kt, t0, t1):
                return (KT[0:96, h, kt * 128:(kt + 1) * 128], QT[qb][0:96, h, t0 - tb * 512:t1 - tb * 512], [dKT[kt], dQT[qb]])

            def kt_range(tb_):
                return list(range(0, 4 * tb_ + 4))

            def ts_range(tb_, kt):
                return (max(0, kt - 4 * tb_), 3)

            def mask_fn(h, kt, lo, hi):
                if kt >= 4 * tb:
                    return [(lo * 128, lo * 128 + 128, c.identb[:], c.ntri[:], [c.dconst])]
                return []

            def v_fn(h, kt):
                return (VA[:, kt, h, :], [dVA[kt]])

            def fin(hg, ts, acc, dacc):
                k.op("dve", lambda e: e.reciprocal(out=rden[:, 0:4], in_=acc[:, :, 64]), reads=[dacc], writes=[drd])
                k.op("dve", lambda e: e.tensor_tensor(out=Ot[:, ts, hg * 256:(hg + 1) * 256].rearrange("p (h d) -> p h d", h=4),
                                                      in0=acc[:, :, 0:64], in1=rden[:, 0:4].unsqueeze(2).to_broadcast([128, 4, 64]),
                                                      op=ALU.mult), reads=[dacc, drd], writes=[dOt[ts]])

            attention(c, ph, 8, 4, qk_fn, kt_range, ts_range, mask_fn, v_fn, 65, fin, tb, PT, dPT,
                      acc_banks=(0, 1, 2, 3), s_banks=(4, 5, 6, 7))
            for ts in range(4):
                i = tb * 4 + ts
                ptr, dptr = bank(c, 6 + (ts % 2))
                ptrb = ptr[:].bitcast(BF16)
                for cc in range(4):
                    k.op("pe", lambda e: e.transpose(out=ptrb[:, cc * 128:(cc + 1) * 128], in_=Ot[:, ts, cc * 128:(cc + 1) * 128],
                                                     identity=c.identb[:]), reads=[dOt[ts], c.dconst], writes=[dptr], signal=(cc == 3))
                k.op("act", lambda e: e.copy(out=XT[:, 4:8, i * 128:(i + 1) * 128], in_=ptrb[:, 0:512].rearrange("p (c t) -> p c t", c=4)),
                     reads=[dptr], writes=[dXT[i]])
        if c.dbg == "p2":
            return dbg_exit(c, s)
        k.barrier()


def dbg_exit(c, s):
    k = c.k
    k.barrier()
    od = Dep()
    k.dma("sp", c.out_d[s, 0:128, :], c.x_d[s, 0:128, :], writes=[od])
    c.outdeps.append(od)
    k.wait_all("sp", c.outdeps)


def rope(c, x, out, cos, sin, tmp, rdeps, dtmp, dout, nh):
    k = c.k
    cb = cos.to_broadcast([128, nh, 16])
    sb_ = sin.to_broadcast([128, nh, 16])
    x1, x2 = x[:, :, 0:16], x[:, :, 16:32]
    k.op("dve", lambda e: e.tensor_tensor(out=tmp[:, :, 0:16], in0=x1, in1=cb, op=ALU.mult), reads=rdeps, writes=[dtmp])
    k.op("dve", lambda e: e.tensor_tensor(out=tmp[:, :, 16:32], in0=x2, in1=sb_, op=ALU.mult), reads=rdeps, writes=[dtmp])
    k.op("dve", lambda e: e.tensor_tensor(out=out[:, :, 0:16], in0=tmp[:, :, 0:16], in1=tmp[:, :, 16:32], op=ALU.subtract),
         reads=[dtmp], writes=[dout])
    k.op("dve", lambda e: e.tensor_tensor(out=tmp[:, :, 0:16], in0=x1, in1=sb_, op=ALU.mult), reads=rdeps, writes=[dtmp])
    k.op("dve", lambda e: e.tensor_tensor(out=tmp[:, :, 16:32], in0=x2, in1=cb, op=ALU.mult), reads=rdeps, writes=[dtmp])
    k.op("dve", lambda e: e.tensor_tensor(out=out[:, :, 16:32], in0=tmp[:, :, 0:16], in1=tmp[:, :, 16:32], op=ALU.add),
         reads=[dtmp], writes=[dout])


def mixer_out(c, s, layer, wo_name, xsrc_d):
    k, nc, di = c.k, c.nc, c.di
    XT, dXT = c.XT, c.dXT
    with contextlib.ExitStack() as ph:
        dW = Dep()
        Wo = k.sb("Wo", [128, 8, D], BF16, ph)
        load_bf16(c, Wo[:], di[wo_name].rearrange("(kc p) n -> p kc n", p=128), dW)
        xin = [k.sb("xin%d" % j, [128, D], F32, ph) for j in range(2)]
        dxin = [Dep(), Dep()]
        for i0 in range(2):
            k.dma("sp", xin[i0][:], xsrc_d[s, i0 * 128:(i0 + 1) * 128, :], reads=[c.dxs[s][i0]], writes=[dxin[i0]])
        G = k.sb("lnG", [128, D], F32, ph); B = k.sb("lnB", [128, D], F32, ph)
        k.dma("sp", G[:], di["ln1_g"][layer:layer + 1, :].partition_broadcast(128), writes=[dW])
        k.dma("sp", B[:], di["ln1_b"][layer:layer + 1, :].partition_broadcast(128), writes=[dW])
        Wr = k.sb("Wr", [128, 8, 36], F32, ph)
        k.dma("sp", Wr[:, :, 0:4], di["moe_w_rg"][layer].rearrange("(kc p) n -> p kc n", p=128), writes=[dW])
        k.dma("sp", Wr[:, :, 4:36], di["moe_w_re"][layer].rearrange("(kc p) n -> p kc n", p=128), writes=[dW])
        Br = k.sb("Br", [128, 36], F32, ph)
        k.dma("sp", Br[:, 0:4], di["moe_b_rg"][layer:layer + 1, :].partition_broadcast(128), writes=[dW])
        k.dma("sp", Br[:, 4:36], di["moe_b_re"][layer:layer + 1, :].partition_broadcast(128), writes=[dW])
        R = [k.sb("R%d" % j, [128, D], F32, ph) for j in range(2)]
        dRr = [Dep(), Dep()]
        X1 = [k.sb("X1%d" % j, [128, D], F32, ph) for j in range(2)]
        dX1 = [Dep(), Dep()]
        xT32 = [k.sb("xT32%d" % j, [128, 8, 128], F32, ph) for j in range(2)]
        dxT32 = [Dep(), Dep()]
        st32, dst = mk_st32(c, ph)
        RT = k.sb("RT", [128, NT, 128], F32, ph); dRT = Dep()
        dL = [Dep() for _ in range(NT)]
        def stageApe(i):
            b = i % 2
            tok = slice(i * 128, (i + 1) * 128)
            if i >= 2:
                k.dma("sp", xin[b][:], xsrc_d[s, tok, :], reads=[c.dxs[s][i]], writes=[dxin[b]])
            pm = [bank(c, 0 + 2 * (i % 3)), bank(c, 1 + 2 * (i % 3))]
            for hf in range(2):
                for kc in range(8):
                    k.op("pe", lambda e: e.matmul(pm[hf][0][:, 0:512], lhsT=XT[:, kc, tok], rhs=Wo[:, kc, hf * 512:(hf + 1) * 512],
                                                  start=(kc == 0), stop=(kc == 7)), reads=[dXT[i], dW], writes=[pm[hf][1]], signal=(kc == 7))

        def stageAdve(i):
            b = i % 2
            pm = [bank(c, 0 + 2 * (i % 3)), bank(c, 1 + 2 * (i % 3))]
            for hf in range(2):
                k.op("dve", lambda e: e.scalar_tensor_tensor(out=R[b][:, hf * 512:(hf + 1) * 512], in0=xin[b][:, hf * 512:(hf + 1) * 512],
                                                             scalar=ALPHA, in1=pm[hf][0][:, 0:512], op0=ALU.mult, op1=ALU.add),
                     reads=[dxin[b], pm[hf][1]], writes=[dRr[b]])
            layer_norm_tile(c, R[b], dRr[b], G, B, dW, X1[b][:], dX1[b], st32, dst, par=i)

        def stageB(i):
            b = i % 2
            tok = slice(i * 128, (i + 1) * 128)
            k.dma("sp", c.xs_d[s, tok, :], X1[b][:], reads=[dX1[b]], writes=[c.dxs[s][i]])
            if c.stop == 2 * layer + 1:
                od = Dep()
                k.dma("sp", c.out_d[s, tok, :], X1[b][:], reads=[dX1[b]], writes=[od])
                c.outdeps.append(od)
            transpose_to_XT(c, X1[b], dX1[b], i, extra32=xT32[b], dextra=dxT32[b], banks=(6, 6))
            pl, dpl = bank(c, 7)
            for kc in range(8):
                k.op("pe", lambda e: e.matmul(pl[:, 0:36], lhsT=xT32[b][:, kc, :], rhs=Wr[:, kc, :], start=(kc == 0), stop=(kc == 7)),
                     reads=[dxT32[b], dW], writes=[dpl], signal=(kc == 7))
            k.op("act", lambda e: e.copy(out=RT[:, i, 0:36], in_=pl[:, 0:36]), reads=[dpl], writes=[dL[i]])

        stageApe(0)
        stageApe(1)
        stageAdve(0)
        for i in range(NT):
            if i + 2 < NT:
                stageApe(i + 2)
            if i + 1 < NT:
                stageAdve(i + 1)
            stageB(i)
        router_all(c, RT, dRT, dL, Br, dW)
        k.barrier()


def router_all(c, RT, dRT, dL, Br, dBr):
    k = c.k
    L = RT[:, :, 0:36]
    f = lambda a, b: RT[:, :, a:b]
    gm, ngs, gw = f(36, 37), f(37, 38), f(38, 39)
    oh, ex = f(40, 44), f(44, 48)
    esel, tmp8, eq1, eq2 = f(48, 56), f(56, 64), f(64, 72), f(72, 80)
    m1, m2, dlt, w1, w2 = f(80, 81), f(81, 82), f(82, 83), f(83, 84), f(84, 85)
    wg = f(88, 120)
    bc = lambda ap, n: ap.to_broadcast([128, NT, n])
    first = [True]

    def V(fn, eng="dve"):
        rd = [dRT] + (list(dL) if first[0] else [])
        first[0] = False
        k.op(eng, fn, reads=rd, writes=[dRT])
    k.op("dve", lambda e: e.tensor_tensor(out=L, in0=L, in1=Br[:].unsqueeze(1).to_broadcast([128, NT, 36]), op=ALU.add),
         reads=[dRT, dBr] + list(dL), writes=[dRT])
    first[0] = False
    V(lambda e: e.tensor_reduce(out=gm, in_=L[:, :, 0:4], axis=AX.X, op=ALU.max))
    V(lambda e: e.tensor_tensor(out=oh, in0=L[:, :, 0:4], in1=bc(gm, 4), op=ALU.is_ge))
    V(lambda e: e.tensor_tensor(out=ex, in0=L[:, :, 0:4], in1=bc(gm, 4), op=ALU.subtract))
    V(lambda e: e.activation(out=ex, in_=ex, func=AF.Exp), "act")
    V(lambda e: e.tensor_reduce(out=ngs, in_=ex, axis=AX.X, op=ALU.add))
    V(lambda e: e.reciprocal(out=gw, in_=ngs))
    V(lambda e: e.tensor_tensor(out=esel, in0=L[:, :, 4:12], in1=bc(oh[:, :, 0:1], 8), op=ALU.mult))
    for g in range(1, 4):
        V(lambda e: e.tensor_tensor(out=tmp8, in0=L[:, :, 4 + 8 * g:12 + 8 * g], in1=bc(oh[:, :, g:g + 1], 8), op=ALU.mult))
        V(lambda e: e.tensor_tensor(out=esel, in0=esel, in1=tmp8, op=ALU.add))
    V(lambda e: e.tensor_reduce(out=m1, in_=esel, axis=AX.X, op=ALU.max))
    V(lambda e: e.tensor_tensor(out=eq1, in0=esel, in1=bc(m1, 8), op=ALU.is_ge))
    V(lambda e: e.tensor_scalar(out=tmp8, in0=eq1, scalar1=-1e30, scalar2=None, op0=ALU.mult))
    V(lambda e: e.tensor_tensor(out=tmp8, in0=tmp8, in1=esel, op=ALU.add))
    V(lambda e: e.tensor_reduce(out=m2, in_=tmp8, axis=AX.X, op=ALU.max))
    V(lambda e: e.tensor_tensor(out=eq2, in0=tmp8, in1=bc(m2, 8), op=ALU.is_ge))
    V(lambda e: e.tensor_tensor(out=dlt, in0=m2, in1=m1, op=ALU.subtract))
    V(lambda e: e.activation(out=dlt, in_=dlt, func=AF.Exp), "act")
    V(lambda e: e.tensor_scalar(out=dlt, in0=dlt, scalar1=1.0, scalar2=None, op0=ALU.add))
    V(lambda e: e.reciprocal(out=w1, in_=dlt))
    V(lambda e: e.tensor_scalar(out=w2, in0=w1, scalar1=-1.0, scalar2=1.0, op0=ALU.mult, op1=ALU.add))
    V(lambda e: e.tensor_tensor(out=w1, in0=w1, in1=gw, op=ALU.mult))
    V(lambda e: e.tensor_tensor(out=w2, in0=w2, in1=gw, op=ALU.mult))
    V(lambda e: e.tensor_tensor(out=eq1, in0=eq1, in1=bc(w1, 8), op=ALU.mult))
    V(lambda e: e.tensor_tensor(out=eq2, in0=eq2, in1=bc(w2, 8), op=ALU.mult))
    V(lambda e: e.tensor_tensor(out=eq1, in0=eq1, in1=eq2, op=ALU.add))
    for g in range(4):
        V(lambda e: e.tensor_tensor(out=wg[:, :, g * 8:(g + 1) * 8], in0=eq1, in1=bc(oh[:, :, g:g + 1], 8), op=ALU.mult))
    k.op("dve", lambda e: e.tensor_copy(out=c.WG[:], in_=wg), reads=[dRT], writes=[c.dWG])


def router_tile(c, i, xT32, dxT32, Wr, Br, dW, rt, drt, wg):
    k = c.k
    pl, dpl = bank(c, 6)
    for kc in range(8):
        k.op("pe", lambda e: e.matmul(pl[:, 0:36], lhsT=xT32[:, kc, :], rhs=Wr[:, kc, :], start=(kc == 0), stop=(kc == 7)),
             reads=[dxT32, dW], writes=[dpl], signal=(kc == 7))
    L = rt[:, 0:36]
    k.op("dve", lambda e: e.tensor_tensor(out=L, in0=pl[:, 0:36], in1=Br[:], op=ALU.add), reads=[dpl, dW], writes=[drt])
    gm = rt[:, 36:37]; oh = rt[:, 40:44]; ex = rt[:, 44:48]; gs = rt[:, 48:49]; gw = rt[:, 49:50]
    esel = rt[:, 52:60]; top8 = rt[:, 60:68]; dlt = rt[:, 68:69]; w1 = rt[:, 69:70]; w2 = rt[:, 70:71]
    m1 = rt[:, 72:80]; m2 = rt[:, 80:88]; we = rt[:, 88:96]; ngm = rt[:, 37:38]
    V = lambda fn, **kw: k.op("dve", fn, reads=[drt], writes=[drt])
    V(lambda e: e.tensor_reduce(out=gm, in_=L[:, 0:4], axis=AX.X, op=ALU.max))
    V(lambda e: e.tensor_scalar(out=oh, in0=L[:, 0:4], scalar1=gm, scalar2=None, op0=ALU.is_ge))
    V(lambda e: e.tensor_scalar(out=ngm, in0=gm, scalar1=-1.0, scalar2=None, op0=ALU.mult))
    k.op("pool", lambda e: e.memset(gs, 0.0), reads=[drt], writes=[drt])
    k.op("act", lambda e: e.activation(out=ex, in_=L[:, 0:4], func=AF.Exp, bias=ngm, scale=1.0, accum_out=gs), reads=[drt], writes=[drt])
    V(lambda e: e.reciprocal(out=gw, in_=gs))
    V(lambda e: e.tensor_scalar(out=esel, in0=L[:, 4:12], scalar1=oh[:, 0:1], scalar2=None, op0=ALU.mult))
    for g in range(1, 4):
        V(lambda e: e.scalar_tensor_tensor(out=esel, in0=L[:, 4 + 8 * g:12 + 8 * g], scalar=oh[:, g:g + 1], in1=esel,
                                           op0=ALU.mult, op1=ALU.add))
    V(lambda e: e.max(out=top8, in_=esel))
    V(lambda e: e.tensor_tensor(out=dlt, in0=top8[:, 1:2], in1=top8[:, 0:1], op=ALU.subtract))
    k.op("act", lambda e: e.activation(out=dlt, in_=dlt, func=AF.Exp), reads=[drt], writes=[drt])
    V(lambda e: e.tensor_scalar(out=dlt, in0=dlt, scalar1=1.0, scalar2=None, op0=ALU.add))
    V(lambda e: e.reciprocal(out=w1, in_=dlt))
    V(lambda e: e.tensor_scalar(out=w2, in0=w1, scalar1=-1.0, scalar2=1.0, op0=ALU.mult, op1=ALU.add))
    V(lambda e: e.tensor_tensor(out=w1, in0=w1, in1=gw, op=ALU.mult))
    V(lambda e: e.tensor_tensor(out=w2, in0=w2, in1=gw, op=ALU.mult))
    V(lambda e: e.tensor_scalar(out=m1, in0=esel, scalar1=top8[:, 0:1], scalar2=w1, op0=ALU.is_ge, op1=ALU.mult))
    V(lambda e: e.tensor_scalar(out=m2, in0=esel, scalar1=top8[:, 1:2], scalar2=w2, op0=ALU.is_equal, op1=ALU.mult))
    V(lambda e: e.tensor_tensor(out=we, in0=m1, in1=m2, op=ALU.add))
    for g in range(4):
        k.op("dve", lambda e: e.tensor_scalar(out=wg[:, g * 8:(g + 1) * 8], in0=we, scalar1=oh[:, g:g + 1], scalar2=None, op0=ALU.mult),
             reads=[drt], writes=[drt])
    pw, dpw = bank(c, 7)
    k.op("pe", lambda e: e.transpose(out=pw[0:32, 0:128], in_=wg[:, 0:32], identity=c.ident[:]), reads=[drt, c.dconst], writes=[dpw])
    k.op("act", lambda e: e.copy(out=c.WGT[:, i * 128:(i + 1) * 128], in_=pw[0:32, 0:128]), reads=[dpw], writes=[c.dWGT[i // 4]])


def moe_prefetch(c, layer, st):
    k, di = c.k, c.di
    NW = 3
    Wg = [k.sb("Wg%d" % j, [128, 8, 256], BF16, st) for j in range(NW)]
    Wu = [k.sb("Wu%d" % j, [128, 8, 256], BF16, st) for j in range(NW)]
    Wd = [k.sb("Wd%d" % j, [128, 2, D], BF16, st) for j in range(NW)]
    dWe = [Dep() for _ in range(NW)]

    def loadw(e_):
        j = e_ % NW
        load_bf16(c, Wg[j][:], di["moe_w_gate"][layer, e_].rearrange("(kc p) n -> p kc n", p=128), dWe[j])
        load_bf16(c, Wu[j][:], di["moe_w_up"][layer, e_].rearrange("(kc p) n -> p kc n", p=128), dWe[j])
        load_bf16(c, Wd[j][:], di["moe_w_down"][layer, e_].rearrange("(kc p) n -> p kc n", p=128), dWe[j])
    for e_ in range(NW):
        loadw(e_)
    return (NW, Wg, Wu, Wd, dWe, loadw)


def moe(c, s, layer, final, W):
    k, nc, di = c.k, c.nc, c.di
    XT, dXT = c.XT, c.dXT
    NW, Wg, Wu, Wd, dWe, loadw = W
    with contextlib.ExitStack() as ph:
        Y = k.sb("Y", [128, NT, D], F32, ph); dY = [Dep() for _ in range(NT)]
        for i in range(NT):
            k.dma("sp", Y[:, i, :], c.xs_d[s, i * 128:(i + 1) * 128, :], reads=[c.dxs[s][i]], writes=[dY[i]])
            k.op("pool", lambda e: e.tensor_scalar(out=Y[:, i, :], in0=Y[:, i, :], scalar1=ALPHA, scalar2=None, op0=ALU.mult),
                 reads=[dY[i]], writes=[dY[i]])
        sg = [k.sb("sg%d" % j, [128, 512], F32, ph) for j in range(2)]; dsg = [Dep(), Dep()]
        wb = [k.sb("wb%d" % j, [128, 512], F32, ph) for j in range(2)]; dwb = [Dep(), Dep()]
        act = [k.sb("act%d" % j, [128, 2, 512], BF16, ph) for j in range(2)]; dact = [Dep(), Dep()]
        items = [(e_, sbk) for e_ in range(32) for sbk in range(4)]
        dn = [4, 5, 6, 7]
        dni = [0]

        def front(n, fs=(0, 1)):
            e_, sbk = items[n]
            j = e_ % NW
            a = n % 2
            t0 = sbk * 512
            for f in fs:
                pg, dpg = bank(c, 0 + f); pu, dpu = bank(c, 2 + f)
                for kc in range(8):
                    k.op("pe", lambda e: e.matmul(pg[:, 0:512], lhsT=Wg[j][:, kc, f * 128:(f + 1) * 128], rhs=XT[:, kc, t0:t0 + 512],
                                                  start=(kc == 0), stop=(kc == 7)),
                         reads=[dWe[j]] + dXT[sbk * 4:sbk * 4 + 4], writes=[dpg], signal=(kc == 7))
                for kc in range(8):
                    k.op("pe", lambda e: e.matmul(pu[:, 0:512], lhsT=Wu[j][:, kc, f * 128:(f + 1) * 128], rhs=XT[:, kc, t0:t0 + 512],
                                                  start=(kc == 0), stop=(kc == 7)),
                         reads=[dWe[j]] + dXT[sbk * 4:sbk * 4 + 4], writes=[dpu], signal=(kc == 7))
                k.op("act", lambda e: e.activation(out=sg[f][:], in_=pg[:, 0:512], func=AF.Silu), reads=[dpg], writes=[dsg[f]])
                k.op("dve", lambda e: e.tensor_tensor(out=act[a][:, f, :], in0=sg[f][:], in1=pu[:, 0:512], op=ALU.mult),
                     reads=[dsg[f], dpu], writes=[dact[a]])

        def back(n, tts=(0, 1, 2, 3)):
            e_, sbk = items[n]
            j = e_ % NW
            a = n % 2
            for tt in tts:
                i = sbk * 4 + tt
                for hf in range(2):
                    po, dpo = bank(c, dn[dni[0] % 4]); dni[0] += 1
                    for f in range(2):
                        k.op("pe", lambda e: e.matmul(po[:, 0:512], lhsT=act[a][:, f, tt * 128:(tt + 1) * 128],
                                                      rhs=Wd[j][:, f, hf * 512:(hf + 1) * 512], start=(f == 0), stop=(f == 1)),
                             reads=[dact[a], dWe[j]], writes=[dpo], signal=(f == 1))
                    k.op("dve", lambda e: e.scalar_tensor_tensor(out=Y[:, i, hf * 512:(hf + 1) * 512], in0=po[:, 0:512],
                                                                 scalar=c.WG[:, i, e_:e_ + 1], in1=Y[:, i, hf * 512:(hf + 1) * 512],
                                                                 op0=ALU.mult, op1=ALU.add),
                         reads=[dpo, dY[i], c.dWG], writes=[dY[i]])
            if sbk == 3 and e_ + 3 < 32 and 3 in tts:
                loadw(e_ + 3)

        front(0)
        for n in range(len(items)):
            if n + 1 < len(items):
                front(n + 1, (0,))
            back(n, (0, 1))
            if n + 1 < len(items):
                front(n + 1, (1,))
            back(n, (2, 3))
        dW = Dep()
        G = k.sb("lnG", [128, D], F32, ph); B = k.sb("lnB", [128, D], F32, ph)
        k.dma("sp", G[:], di["ln2_g"][layer:layer + 1, :].partition_broadcast(128), writes=[dW])
        k.dma("sp", B[:], di["ln2_b"][layer:layer + 1, :].partition_broadcast(128), writes=[dW])
        st32, dst = mk_st32(c, ph)
        def lnA(i):
            Yi = Y[:, i, :]
            layer_norm_tile(c, _V(Yi), dY[i], G, B, dW, Yi, dY[i], st32, dst, par=i)

        def lnB(i):
            tok = slice(i * 128, (i + 1) * 128)
            Yi = Y[:, i, :]
            if final:
                od = Dep()
                k.dma("sp", c.out_d[s, tok, :], Yi, reads=[dY[i]], writes=[od])
                c.outdeps.append(od)
            else:
                k.dma("sp", c.xs_d[s, tok, :], Yi, reads=[dY[i]], writes=[c.dxs[s][i]])
                transpose_to_XT(c, _V(Yi), dY[i], i, banks=(0 + 2 * (i % 2), 1 + 2 * (i % 2)))

        lnA(0)
        for i in range(NT):
            if i + 1 < NT:
                lnA(i + 1)
            lnB(i)
        k.barrier()


class _V:
    def __init__(self, ap):
        self.ap = ap

    def __getitem__(self, key):
        if key == slice(None):
            return self.ap
        return self.ap[key]


def layer1(c, s):
    k, nc, di = c.k, c.nc, c.di
    XT, dXT = c.XT, c.dXT
    with contextlib.ExitStack() as ph:
        p1 = ph.enter_context(contextlib.ExitStack())
        dW = Dep()
        KD = {}
        dKD = {}
        for nm in ("s", "w"):
            for g in range(2):
                for hf in range(1):
                    KD[nm, g, hf] = k.sb("KZ%s%d%d" % (nm, g, hf), [128, S], BF16, ph)
                    k.op("pool", lambda e: e.memset(KD[nm, g, hf][64:128, :], 0.0), writes=[dW])
                dKD[nm, g] = [Dep() for _ in range(4)]
        VS = k.sb("VS", [128, NT, 2, 65], BF16, ph); dVS = [Dep() for _ in range(NT)]
        VW = k.sb("VW", [128, NT, 2, 65], BF16, ph); dVW = [Dep() for _ in range(NT)]
        GATE = k.sb("GATE", [128, NT, 48], F32, ph); dG = [Dep() for _ in range(NT)]
        KC2 = [[k.sb("KC2%d%d" % (g, hf), [128, 128], BF16, ph) for hf in range(2)] for g in range(2)]; dKC = [Dep(), Dep()]
        VCA = [k.sb("VCA%d" % g, [128, 97], BF16, ph) for g in range(2)]; dVC = [Dep(), Dep()]
        k.op("pool", lambda e: e.memset(VS[:], 1.0), writes=dVS)
        k.op("pool", lambda e: e.memset(VW[:], 1.0), writes=dVW)
        cov32 = k.sb("cov32", [128, 32], F32, ph)
        k.dma("sp", cov32[:], di["cover"][:, :], writes=[dW])
        for g in range(2):
            for hf in range(2):
                k.op("pool", lambda e: e.memset(KC2[g][hf][:], 0.0), writes=[dKC[g]])
            k.op("pool", lambda e: e.memset(VCA[g][:], 0.0), writes=[dVC[g]])
            k.op("pool", lambda e: e.memset(VCA[g][0:127, 64:65], 1.0), reads=[dVC[g]], writes=[dVC[g]])
            k.op("dve", lambda e: e.tensor_copy(out=VCA[g][:, 65:97], in_=cov32[:]), reads=[dW, dVC[g]], writes=[dVC[g]])
        Cin = k.sb("Cin", [128, 8, 1840], BF16, ph)
        dWq = Dep()
        cw = di["c_w_in"].rearrange("(kc p) n -> p kc n", p=128)
        base = {"c": 1024, "v": 1152, "s": 1280, "w": 1536}
        dCk = Dep()
        load_bf16(c, Cin[:, :, 1024:1840], cw[:, :, 1024:1840], dCk)
        Wd = {}
        ei = 0
        for nm in ("c", "v", "s", "w"):
            for g in range(2):
                Wd[nm, g] = k.sb("Wd%s%d" % (nm, g), [128, 8, 128], BF16, p1)
                for hh in range(2):
                    eng = ("dve", "act", "pool")[ei % 3]; ei += 1
                    src = Cin[:, :, base[nm] + g * 64:base[nm] + (g + 1) * 64]
                    dst = Wd[nm, g][:, :, hh * 64:(hh + 1) * 64]
                    if eng == "act":
                        k.op("act", lambda e: e.copy(out=dst, in_=src), reads=[dCk], writes=[dW])
                    else:
                        k.op(eng, lambda e: e.tensor_copy(out=dst, in_=src), reads=[dCk], writes=[dW])
        W1 = {}
        W2k = k.sb("W2k", [128, 2, 128], BF16, p1)
        W2v = k.sb("W2v", [128, 2, 64], BF16, p1)
        for kv, nm1 in (("c", "c_w_ck1"), ("v", "c_w_cv1")):
            W1[kv] = k.sb("W1" + kv, [128, 16, 256], BF16, p1)
            w1v = di[nm1].rearrange("(h l d) c -> h d l c", h=2, l=16, d=64)
            for hh in range(2):
                load_bf16(c, W1[kv][hh * 64:(hh + 1) * 64, :, :], w1v[hh], dW)
        w2kv = di["c_w_ck2"].rearrange("(cc p) d -> p cc d", p=128)
        for hh in range(2):
            load_bf16(c, W2k[:, :, hh * 64:(hh + 1) * 64], w2kv, dW)
        load_bf16(c, W2v[:], di["c_w_cv2"].rearrange("(cc p) d -> p cc d", p=128), dW)
        PE2 = k.sb("PE2", [128, 2, 16], F32, p1)
        k.dma("sp", PE2[:], di["cmp_posT"].rearrange("k p l -> p k l"), writes=[dW])
        gb = k.sb("gb", [128, 48], F32, p1)
        k.dma("sp", gb[:], di["c_gate_b"][0:1, :].partition_broadcast(128), writes=[dW])
        A2 = {}
        dA2 = {}
        for kv in ("c", "v"):
            for g in range(2):
                A2[kv, g] = k.sb("A2%s%d" % (kv, g), [128, S], BF16, p1)
                dA2[kv, g] = Dep()
                k.op("pool", lambda e: e.memset(A2[kv, g][:, S - 16:S], 0.0), writes=[dA2[kv, g]])
        load_bf16(c, Cin[:, :, 0:1024], cw[:, :, 0:1024], dWq)
        gtmp = k.sb("gtmp", [128, 48], F32, p1); dgt = Dep()
        rr = 0
        for tb in range(4):
            t0 = tb * 512
            tsl = slice(t0, t0 + 512)
            xr = dXT[tb * 4:tb * 4 + 4]
            for nm in ("c", "v", "s", "w"):
                for g in range(2):
                    pb_, dpb_ = bank(c, 4 + rr % 4); rr += 1
                    for kc in range(8):
                        k.op("pe", lambda e: e.matmul(pb_[:, 0:512], lhsT=Wd[nm, g][:, kc, :], rhs=XT[:, kc, tsl],
                                                      start=(kc == 0), stop=(kc == 7)), reads=[dW] + xr, writes=[dpb_], signal=(kc == 7))
                    if nm in ("s", "w"):
                        k.op("dve", lambda e: e.tensor_copy(out=KD[nm, g, 0][0:64, tsl], in_=pb_[0:64, 0:512]), reads=[dpb_, dW], writes=[dKD[nm, g][tb]])
                    else:
                        kvi = 0 if nm == "c" else 1
                        k.op("dve", lambda e: e.tensor_tensor(out=A2[nm, g][0:64, tsl].rearrange("p (n l) -> p n l", l=16),
                                                              in0=pb_[0:64, 0:512].rearrange("p (n l) -> p n l", l=16),
                                                              in1=PE2[0:64, kvi, :].unsqueeze(1).to_broadcast([64, 32, 16]), op=ALU.add),
                             reads=[dpb_, dW], writes=[dA2[nm, g]])
                        lo = 16 if tb == 0 else 0
                        nb = (512 - lo) // 16
                        k.op("dve", lambda e: e.tensor_tensor(out=A2[nm, g][64:128, t0 - 16 + lo:t0 + 496].rearrange("p (n l) -> p n l", l=16),
                                                              in0=pb_[64:128, lo:512].rearrange("p (n l) -> p n l", l=16),
                                                              in1=PE2[64:128, kvi, :].unsqueeze(1).to_broadcast([64, nb, 16]), op=ALU.add),
                             reads=[dpb_, dW], writes=[dA2[nm, g]])
            for ts in range(4):
                i = tb * 4 + ts
                tok = slice(i * 128, (i + 1) * 128)
                pb_, dpb_ = bank(c, rr % 4); rr += 1
                for kc in range(8):
                    k.op("pe", lambda e: e.matmul(pb_[:, 0:128], lhsT=XT[:, kc, tok], rhs=Cin[:, kc, 1408:1536], start=(kc == 0), stop=(kc == 7)),
                         reads=[dW, dCk, dXT[i]], writes=[dpb_], signal=False)
                for kc in range(8):
                    k.op("pe", lambda e: e.matmul(pb_[:, 128:304], lhsT=XT[:, kc, tok], rhs=Cin[:, kc, 1664:1840], start=(kc == 0), stop=(kc == 7)),
                         reads=[dW, dCk, dXT[i]], writes=[dpb_], signal=(kc == 7))
                k.op("act", lambda e: e.copy(out=VS[:, i, :, 0:64], in_=pb_[:, 0:128].rearrange("p (g d) -> p g d", g=2)), reads=[dpb_], writes=[dVS[i]])
                k.op("act", lambda e: e.copy(out=VW[:, i, :, 0:64], in_=pb_[:, 128:256].rearrange("p (g d) -> p g d", g=2)), reads=[dpb_], writes=[dVW[i]])
                k.op("act", lambda e: e.copy(out=gtmp[:], in_=pb_[:, 256:304]), reads=[dpb_], writes=[dgt])
                k.op("dve", lambda e: e.tensor_tensor(out=gtmp[:], in0=gtmp[:], in1=gb[:], op=ALU.add), reads=[dgt, dW], writes=[dgt])
                k.op("act", lambda e: e.activation(out=GATE[:, i, :], in_=gtmp[:], func=AF.Sigmoid), reads=[dgt], writes=[dG[i]])
        GT = k.sb("GT", [128, 2, 128], BF16, p1); dGT = Dep()
        for kv in ("c", "v"):
            for g in range(2):
                ph_, dph_ = bank(c, 0 if kv == "c" else 1)
                a2v = A2[kv, g][:, :].rearrange("p (n l) -> p n l", l=16)
                for cc in range(2):
                    for l in range(16):
                        k.op("pe", lambda e: e.matmul(ph_[:, cc * 128:cc * 128 + 127], lhsT=W1[kv][:, l, cc * 128:(cc + 1) * 128],
                                                      rhs=a2v[:, 0:127, l], start=(l == 0), stop=(l == 15)),
                             reads=[dW, dA2[kv, g]], writes=[dph_], signal=(l == 15 and cc == 1))
                k.op("act", lambda e: e.activation(out=GT[:, :, 0:127], in_=ph_[:, 0:256].rearrange("p (c n) -> p c n", c=2)[:, :, 0:127],
                                                   func=AF.Gelu_apprx_tanh), reads=[dph_], writes=[dGT])
                po_, dpo_ = bank(c, 2 if kv == "c" else 3)
                if kv == "c":
                    for cc in range(2):
                        k.op("pe", lambda e: e.matmul(po_[:, 0:127], lhsT=W2k[:, cc, :], rhs=GT[:, cc, 0:127], start=(cc == 0), stop=(cc == 1)),
                             reads=[dW, dGT], writes=[dpo_], signal=(cc == 1))
                    k.op("act", lambda e: e.copy(out=KC2[g][0][0:64, 0:127], in_=po_[0:64, 0:127]), reads=[dpo_, dKC[g]], writes=[dKC[g]])
                    k.op("act", lambda e: e.copy(out=KC2[g][1][64:128, 0:127], in_=po_[64:128, 0:127]), reads=[dpo_, dKC[g]], writes=[dKC[g]])
                else:
                    for cc in range(2):
                        k.op("pe", lambda e: e.matmul(po_[0:127, 0:64], lhsT=GT[:, cc, 0:127], rhs=W2v[:, cc, :], start=(cc == 0), stop=(cc == 1)),
                             reads=[dW, dGT], writes=[dpo_], signal=(cc == 1))
                    k.op("act", lambda e: e.copy(out=VCA[g][0:127, 0:64], in_=po_[0:127, 0:64]), reads=[dpo_, dVC[g]], writes=[dVC[g]])
        k.barrier()
        p1.close()
        cm32 = k.sb("cm32", [128, S], F32, ph)
        cmask = k.sb("cmask", [128, S], BF16, ph)
        dC = Dep()
        k.dma("sp", cm32[:], di["cmask"][:, :], writes=[dC])
        k.op("pool", lambda e: e.tensor_scalar(out=cmask[:], in0=cm32[:], scalar1=-1.0, scalar2=30000.0, op0=ALU.add, op1=ALU.mult),
             reads=[dC], writes=[dC])
        cand = k.sb("cand", [128, NT * 32], F32, ph); sbias = k.sb("sbias", [128, NT * 32], F32, ph)
        k.dma("sp", cand[:], di["cand"][:, :], writes=[dC])
        k.dma("sp", sbias[:], di["sbias"][:, :], writes=[dC])
        for g in range(2):
            c.k.dma("pool", KD["s", g, 0][64:96, :], di["e2"][:, :], writes=dKD["s", g])
        Qe = k.sb("Qe", [128, 8, 512], BF16, ph); Qo = k.sb("Qo", [128, 8, 512], BF16, ph)
        QQ = (Qe, Qo)
        dQq = Dep(); dQs = [Dep(), Dep()]
        k.op("pool", lambda e: e.memset(Qe[64:128, :, :], 0.0), writes=dQs)
        k.op("pool", lambda e: e.memset(Qo[64:128, :, :], 0.0), writes=dQs)
        qrr = [0]
        O = k.sb("O", [128, 4, D], F32, ph); dO = [Dep() for _ in range(4)]
        PT = [k.sb("PT%d" % j, [128, 512], BF16, ph) for j in range(6)]; dPT = [Dep() for _ in range(6)]
        imp = k.sb("imp", [128, 4, 32], F32, ph); dimp = [Dep() for _ in range(4)]
        sm = k.sb("sm1", [128, 64], F32, ph); dsm = Dep()
        otmp = k.sb("otmp", [128, 4, 64], F32, ph); dot = Dep()
        selb = k.sb("selb", [128, 32], BF16, ph)
        for tb in range(4):
            tsl = slice(tb * 512, (tb + 1) * 512)
            for cq in range(8):
                for par in range(2):
                    pb_, dpb_ = bank(c, 4 + qrr[0] % 4); qrr[0] += 1
                    for kc in range(8):
                        k.op("pe", lambda e: e.matmul(pb_[0:64, 0:512], lhsT=Cin[:, kc, cq * 128 + par * 64:cq * 128 + par * 64 + 64], rhs=XT[:, kc, tsl],
                                                      start=(kc == 0), stop=(kc == 7)), reads=[dWq] + dXT[tb * 4:tb * 4 + 4], writes=[dpb_], signal=(kc == 7))
                    k.op("act", lambda e: e.activation(out=QQ[par][0:64, cq, :], in_=pb_[0:64, 0:512], func=AF.Copy, scale=0.125),
                         reads=[dpb_], writes=[dQq])
            for g in range(2):
                def q_ap(h, rows, t0, t1):
                    H = g * 8 + h
                    return QQ[H % 2][0:rows, H // 2, t0 - tb * 512:t1 - tb * 512]

                def fin_gen(br, first):
                    def fin(hg, ts, acc, dacc):
                        i = tb * 4 + ts
                        den, rden, coef = sm[:, 0:4], sm[:, 4:8], sm[:, 8:12]
                        k.op("dve", lambda e: e.tensor_scalar(out=den, in0=acc[:, :, 64], scalar1=1e-30, scalar2=None, op0=ALU.max),
                             reads=[dacc, dsm], writes=[dsm])
                        k.op("dve", lambda e: e.reciprocal(out=rden, in_=den), reads=[dsm], writes=[dsm])
                        g0 = br * 16 + g * 8 + hg * 4
                        k.op("dve", lambda e: e.tensor_tensor(out=coef, in0=rden, in1=GATE[:, i, g0:g0 + 4], op=ALU.mult),
                             reads=[dsm, dG[i]], writes=[dsm])
                        c0 = (g * 8 + hg * 4) * 64
                        Ov = O[:, ts, c0:c0 + 256].rearrange("p (h d) -> p h d", h=4)
                        cb = coef.unsqueeze(2).to_broadcast([128, 4, 64])
                        if first:
                            k.op("dve", lambda e: e.tensor_tensor(out=Ov, in0=acc[:, :, 0:64], in1=cb, op=ALU.mult),
                                 reads=[dacc, dsm], writes=[dO[ts]])
                            for h in range(4):
                                if hg == 0 and h == 0:
                                    k.op("dve", lambda e: e.tensor_scalar(out=imp[:, ts, :], in0=acc[:, h, 65:97], scalar1=rden[:, h:h + 1],
                                                                          scalar2=None, op0=ALU.mult), reads=[dacc, dsm], writes=[dimp[ts]])
                                else:
                                    k.op("dve", lambda e: e.scalar_tensor_tensor(out=imp[:, ts, :], in0=acc[:, h, 65:97], scalar=rden[:, h:h + 1],
                                                                                 in1=imp[:, ts, :], op0=ALU.mult, op1=ALU.add),
                                         reads=[dacc, dsm, dimp[ts]], writes=[dimp[ts]])
                        else:
                            k.op("dve", lambda e: e.tensor_tensor(out=otmp[:], in0=acc[:, :, 0:64], in1=cb, op=ALU.mult),
                                 reads=[dacc, dsm], writes=[dot])
                            k.op("pool", lambda e: e.tensor_tensor(out=Ov, in0=Ov, in1=otmp[:], op=ALU.add), reads=[dot, dO[ts]], writes=[dO[ts]])
                    return fin

                def mask_cmp(h, kt, lo, hi):
                    return [(0, 512, c.identb[:], cmask[:, tb * 512:(tb + 1) * 512], [dC, c.dconst])]
                attention(c, ph, 8, 4, (lambda h, kt, t0, t1: (KC2[g][0][0:64, :], q_ap(h, 64, t0, t1), [dKC[g], dQq])),
                          lambda tb_: [0], lambda tb_, kt: (0, 3), mask_cmp, lambda h, kt: (VCA[g][:, :], [dVC[g]]), 97,
                          fin_gen(0, True), tb, PT, dPT, acc_banks=(0, 1, 2, 3), s_banks=(4, 5, 6))
                for ts in range(4):
                    i = tb * 4 + ts
                    sc, top8, selm_ = sm[:, 16:48], sm[:, 48:56], sm[:, 16:48]
                    k.op("dve", lambda e: e.tensor_tensor(out=sc, in0=imp[:, ts, :], in1=cand[:, i * 32:(i + 1) * 32], op=ALU.mult),
                         reads=[dimp[ts], dC, dsm], writes=[dsm])
                    k.op("dve", lambda e: e.tensor_tensor(out=sc, in0=sc, in1=sbias[:, i * 32:(i + 1) * 32], op=ALU.add), reads=[dsm, dC], writes=[dsm])
                    k.op("dve", lambda e: e.max(out=top8, in_=sc), reads=[dsm], writes=[dsm])
                    k.op("dve", lambda e: e.tensor_scalar(out=selb[:], in0=sc, scalar1=top8[:, 7:8], scalar2=None, op0=ALU.is_ge),
                         reads=[dsm], writes=[dsm])
                    k.op("dve", lambda e: e.tensor_scalar(out=selb[:], in0=selb[:], scalar1=-1.0, scalar2=30000.0, op0=ALU.add, op1=ALU.mult),
                         reads=[dsm], writes=[dsm])
                    pt_, dpt_ = bank(c, 7)
                    ptb = pt_[:].bitcast(BF16)
                    k.op("pe", lambda e: e.transpose(out=ptb[64:96, 0:128], in_=selb[:, :], identity=c.identb[:]), reads=[dsm, c.dconst], writes=[dpt_])
                    for par in range(2):
                        k.op("act", lambda e: e.copy(out=QQ[par][64:96, g * 4:(g + 1) * 4, ts * 128:(ts + 1) * 128],
                                                     in_=ptb[64:96, 0:128].unsqueeze(1).to_broadcast([32, 4, 128])), reads=[dpt_], writes=[dQs[g]])

                def mask_sel(h, kt, lo, hi):
                    c0 = lo * 128
                    if kt >= 4 * tb:
                        return [(c0, c0 + 128, c.identb[:], c.ntri[:], [c.dconst])]
                    return []
                attention(c, ph, 8, 4, (lambda h, kt, t0, t1: (KD["s", g, 0][0:96, kt * 128:(kt + 1) * 128], q_ap(h, 96, t0, t1),
                                                              [dKD["s", g][kt // 4], dQq, dQs[g]])),
                          lambda tb_: list(range(0, 4 * tb_ + 4)), lambda tb_, kt: (max(0, kt - 4 * tb_), 3), mask_sel,
                          lambda h, kt: (VS[:, kt, g, :], [dVS[kt]]), 65, fin_gen(1, False), tb, PT, dPT, acc_banks=(0, 1, 2, 3), s_banks=(4, 5, 6))

                def mask_win(h, kt, lo, hi):
                    res = []
                    for ts in range(lo, hi + 1):
                        dl = 4 * tb + ts - kt
                        if dl == 0:
                            res.append((ts * 128, ts * 128 + 128, c.identb[:], c.ntri[:], [c.dconst]))
                        elif dl == 4:
                            res.append((ts * 128, ts * 128 + 128, c.identb[:], c.ntri2[:], [c.dconst]))
                    return res
                attention(c, ph, 8, 4, (lambda h, kt, t0, t1: (KD["w", g, 0][:, kt * 128:(kt + 1) * 128], q_ap(h, 128, t0, t1),
                                                              [dKD["w", g][kt // 4], dQq, dQs[g]])),
                          lambda tb_: list(range(max(0, 4 * tb_ - 4), 4 * tb_ + 4)),
                          lambda tb_, kt: (max(0, kt - 4 * tb_), min(3, kt - 4 * tb_ + 4)), mask_win,
                          lambda h, kt: (VW[:, kt, g, :], [dVW[kt]]), 65, fin_gen(2, False), tb, PT, dPT, acc_banks=(0, 1, 2, 3), s_banks=(4, 5, 6))
            for ts in range(4):
                i = tb * 4 + ts
                transpose_to_XT(c, _V(O[:, ts, :]), dO[ts], i, banks=(6, 7))
        k.barrier()


def host_consts():
    h = {}
    h["ident"] = np.eye(128, dtype=np.float32)
    kk = np.arange(128)[:, None]; tt = np.arange(128)[None, :]
    h["tri"] = (kk <= tt).astype(np.float32)
    h["tri2"] = (kk > tt).astype(np.float32)
    fr = np.exp(-math.log(10000.0) * np.arange(16, dtype=np.float32) / 16).astype(np.float32)
    h["freq"] = np.tile((fr / np.float32(2 * math.pi)).astype(np.float32)[None, :], (128, 1))
    sel = np.zeros((32, 32, 128), np.float32)
    for e in range(32):
        sel[e, e, :] = 1.0
    h["selm"] = sel.reshape(32, 32 * 128)
    n = np.arange(128)[:, None]; t = np.arange(S)[None, :]
    h["cmask"] = ((t >= 16 * n + 31) & (n < 127)).astype(np.float32)
    c0 = np.arange(128)[:, None] * 16; s0 = np.arange(32)[None, :] * 64
    h["cover"] = ((c0 < s0 + 64) & (c0 + 32 > s0) & (np.arange(128)[:, None] < 127)).astype(np.float32)
    tok = np.arange(S); tb = tok // 64; jj = np.arange(32)[None, :]
    forced = (jj == 0) | (jj == tb[:, None]) | (jj == tb[:, None] - 1)
    cand = (~forced) & (jj <= tb[:, None])
    sbias = np.where(forced, 1e4, np.where(jj <= tb[:, None], 0.0, -1e4)).astype(np.float32)
    h["cand"] = cand.astype(np.float32).reshape(NT, 128, 32).transpose(1, 0, 2).reshape(128, NT * 32).copy()
    h["sbias"] = sbias.reshape(NT, 128, 32).transpose(1, 0, 2).reshape(128, NT * 32).copy()
    e2 = np.zeros((32, NT, 128), np.float32)
    for kt in range(NT):
        for m in range(128):
            e2[2 * kt + m // 64, kt, m] = 1.0
    h["e2"] = e2.reshape(32, NT * 128)
    return h


def host_inputs(inputs, seqs):
    f = lambda a: np.ascontiguousarray(np.asarray(a, dtype=np.float32))
    m = {}
    m["x"] = f(inputs["x"][seqs])
    pos = np.asarray(inputs["positions"])[seqs].astype(np.int32)
    m["posT"] = np.ascontiguousarray(pos.reshape(len(seqs), NT, 128).transpose(2, 0, 1).reshape(128, len(seqs) * NT))
    m["ab_w_in"] = f(inputs["ab_w_in"][0]); m["gm_ln_g"] = f(inputs["ab_gm_ln_g"]); m["gm_ln_b"] = f(inputs["ab_gm_ln_b"])
    m["gm_wsT"] = f(np.asarray(inputs["ab_gm_ws"][0]).transpose(0, 2, 1)); m["gm_bs"] = f(np.asarray(inputs["ab_gm_bs"][0]).reshape(1, 512))
    m["qg2"] = f(np.asarray(inputs["ab_mla_q_norm"][0]).reshape(2, 128).T); m["kvg"] = f(np.asarray(inputs["ab_mla_kv_norm"][0]).reshape(128, 1))
    m["w_uq"] = f(inputs["ab_mla_w_uq"][0]); m["w_uk"] = f(inputs["ab_mla_w_uk"][0]); m["w_uv"] = f(inputs["ab_mla_w_uv"][0])
    m["ab_w_o"] = f(inputs["ab_w_o"][0]); m["c_w_in"] = f(inputs["c_w_in"][0])
    cp = np.asarray(inputs["c_cmp_pos"][0])
    m["cmp_posT"] = f(cp.reshape(2, 2, 16, 64).transpose(0, 1, 3, 2).reshape(2, 128, 16))
    for n_ in ("c_w_ck1", "c_w_ck2", "c_w_cv1", "c_w_cv2", "c_w_o"):
        m[n_] = f(inputs[n_][0])
    m["c_gate_b"] = f(np.asarray(inputs["c_gate_b"]).reshape(1, 48))
    for n_ in ("moe_w_rg", "moe_b_rg", "moe_w_re", "moe_b_re", "moe_w_gate", "moe_w_up", "moe_w_down", "ln1_g", "ln1_b", "ln2_g", "ln2_b"):
        m[n_] = f(inputs[n_])
    m.update(host_consts())
    return m


_CACHE = {}


def kernel(**inputs):
    ncores, nseq = 8, 2
    if "nc" not in _CACHE:
        _CACHE["nc"] = build(nseq)[0]
    nc = _CACHE["nc"]
    base = host_inputs(inputs, [0, 1])
    in_maps = []
    x = np.asarray(inputs["x"], dtype=np.float32)
    pos = np.asarray(inputs["positions"]).astype(np.int32)
    for cid in range(ncores):
        m = dict(base)
        seqs = [2 * cid, 2 * cid + 1]
        m["x"] = np.ascontiguousarray(x[seqs])
        m["posT"] = np.ascontiguousarray(pos[seqs].reshape(nseq, NT, 128).transpose(2, 0, 1).reshape(128, nseq * NT))
        in_maps.append(m)
    res = run_bass_kernel_spmd(nc, in_maps, core_ids=list(range(ncores)))
    out = np.concatenate([np.asarray(r["out"]) for r in res.results], axis=0)
    return out.astype(np.float32)
```

```python
import contextlib
import math
import numpy as np
import concourse.bass as bass
import concourse.mybir as mybir
from concourse.bass_utils import run_bass_kernel_spmd

F32 = mybir.dt.float32
BF16 = mybir.dt.bfloat16
I32 = mybir.dt.int32
AF = mybir.ActivationFunctionType
ALU = mybir.AluOpType
AX = mybir.AxisListType
NDS = 24
S = 2048
D = 1024
NT = 16
ALPHA = 4.0 ** 0.25
EPS = 1e-5


class Dep:
    __slots__ = ("w", "r", "excl")

    def __init__(self, excl=False):
        self.w = None
        self.r = {}
        self.excl = excl


class KB:
    def __init__(self, nc, stack):
        self.nc = nc
        self.stack = stack
        self.E = {"pe": nc.tensor, "act": nc.scalar, "dve": nc.vector,
                  "pool": nc.gpsimd, "sp": nc.sync}
        self.sem = {e: stack.enter_context(nc.semaphore(e + "_sem"))
                    for e in ("pe", "act", "dve", "pool")}
        self.cnt = {e: 0 for e in self.sem}
        self.known = {e: {} for e in self.E}
        self.dsem = [stack.enter_context(nc.semaphore("dq%d" % i)) for i in range(NDS)]
        self.dcnt = [0] * NDS
        self.dnext = 0
        self.dnextp = 0
        self.nins = 0

    def _wait(self, eng, evs):
        e = self.E[eng]
        need = {}
        for ev in evs:
            if ev is None:
                continue
            s, v = ev
            if eng == "pe" and s is self.sem["pe"]:
                continue
            if self.known[eng].get(id(s), 0) >= v:
                continue
            if eng in self.sem and s is self.sem[eng]:
                assert v <= self.cnt[eng], "same-engine dep on unsignaled op (%s)" % eng
            if need.get(id(s), (None, 0))[1] < v:
                need[id(s)] = (s, v)
        for s, v in need.values():
            e.wait_ge(s, v)
            self.known[eng][id(s)] = v

    def _evs(self, reads, writes, eng=None):
        evs = []
        own = self.sem.get(eng)
        for d in reads:
            evs.append(d.w)
            if d.excl:
                evs.extend(ev for ev in d.r.values() if ev[0] is not own)
        for d in writes:
            evs.append(d.w)
            evs.extend(d.r.values())
        return evs

    @staticmethod
    def _upd(ev, reads, writes):
        s, v = ev
        for d in reads:
            o = d.r.get(id(s))
            if o is None or o[1] < v:
                d.r[id(s)] = ev
        for d in writes:
            d.w = ev
            d.r = {}

    def op(self, eng, fn, reads=(), writes=(), signal=True):
        self._wait(eng, self._evs(reads, writes, eng))
        ins = fn(self.E[eng])
        self.nins += 1
        if signal:
            self.cnt[eng] += 1
            ins.then_inc(self.sem[eng], 1)
            ev = (self.sem[eng], self.cnt[eng])
        else:
            ev = (self.sem[eng], self.cnt[eng] + 1)
        self._upd(ev, reads, writes)
        return ev

    def dma(self, q, out, in_, reads=(), writes=(), **kw):
        if q == "pool":
            j = 16 + self.dnextp
            self.dnextp = (self.dnextp + 1) % (NDS - 16)
        else:
            j = self.dnext
            self.dnext = (j + 1) % 16
        evs = self._evs(reads, writes)
        if self.dcnt[j] > 0:
            evs.append((self.dsem[j], self.dcnt[j]))
        self._wait(q, evs)
        ins = self.E[q].dma_start(out=out, in_=in_, **kw)
        self.nins += 1
        self.dcnt[j] += 16
        ins.then_inc(self.dsem[j], 16)
        ev = (self.dsem[j], self.dcnt[j])
        self._upd(ev, reads, writes)
        return ev

    def barrier(self):
        evs = [(self.sem[e], self.cnt[e]) for e in self.sem if self.cnt[e] > 0]
        evs += [(self.dsem[j], self.dcnt[j]) for j in range(NDS) if self.dcnt[j] > 0]
        for eng in self.E:
            self._wait(eng, evs)

    def wait_all(self, eng, deps):
        evs = []
        for d in deps:
            evs.append(d.w)
            evs.extend(d.r.values())
        self._wait(eng, evs)

    def sb(self, name, shape, dt, stack=None):
        self.nsb = getattr(self, "nsb", 0) + 1
        return (stack or self.stack).enter_context(self.nc.sbuf_tensor("%s_s%d" % (name, self.nsb), list(shape), dt))


class Ctx:
    pass


def build(nseq=2, stop=99):
    nc = bass.Bass("TRN2", target_bir_lowering=False)
    di = {}

    def inp(name, shape, dt=F32):
        di[name] = nc.dram_tensor(name, list(shape), dt, kind="ExternalInput").ap()
        return di[name]

    x_d = inp("x", [nseq, S, D])
    posT_d = inp("posT", [128, nseq * NT], I32)
    inp("ab_w_in", [D, 1440]); inp("gm_ln_g", [1, 512]); inp("gm_ln_b", [1, 512])
    inp("gm_wsT", [4, 128, 128]); inp("gm_bs", [1, 512])
    inp("qg2", [128, 2]); inp("kvg", [128, 1])
    inp("w_uq", [256, 768]); inp("w_uk", [128, 512]); inp("w_uv", [128, 512]); inp("ab_w_o", [D, D])
    inp("c_w_in", [D, 1840]); inp("cmp_posT", [2, 128, 16])
    inp("c_w_ck1", [2048, 256]); inp("c_w_ck2", [256, 64]); inp("c_w_cv1", [2048, 256]); inp("c_w_cv2", [256, 64])
    inp("c_gate_b", [1, 48]); inp("c_w_o", [D, D])
    inp("moe_w_rg", [2, D, 4]); inp("moe_b_rg", [2, 4]); inp("moe_w_re", [2, D, 32]); inp("moe_b_re", [2, 32])
    inp("moe_w_gate", [2, 32, D, 256]); inp("moe_w_up", [2, 32, D, 256]); inp("moe_w_down", [2, 32, 256, D])
    inp("ln1_g", [2, D]); inp("ln1_b", [2, D]); inp("ln2_g", [2, D]); inp("ln2_b", [2, D])
    inp("ident", [128, 128]); inp("tri", [128, 128]); inp("tri2", [128, 128]); inp("freq", [128, 16])
    inp("selm", [32, 32 * 128]); inp("cmask", [128, S]); inp("cover", [128, 32])
    inp("cand", [128, NT * 32]); inp("sbias", [128, NT * 32]); inp("e2", [32, NT * 128])
    out_d = nc.dram_tensor("out", [nseq, S, D], F32, kind="ExternalOutput").ap()
    xs_d = nc.dram_tensor("xscr", [nseq, S, D], F32, kind="Internal").ap()

    with contextlib.ExitStack() as st:
        k = KB(nc, st)
        c = Ctx()
        c.nc, c.k, c.di, c.nseq = nc, k, di, nseq
        c.x_d, c.out_d, c.xs_d, c.posT_d = x_d, out_d, xs_d, posT_d
        c.stop = stop
        import os
        c.dbg = os.environ.get("KDBG", "")
        c.ps = [st.enter_context(nc.psum_tensor("ps%d" % i, [128, 512], F32)) for i in range(8)]
        c.dps = [Dep(excl=True) for _ in range(8)]
        c.rr = 0
        c.XT = k.sb("XT", [128, 8, S], BF16); c.dXT = [Dep() for _ in range(NT)]
        c.ident = k.sb("ident", [128, 128], F32); c.identb = k.sb("identb", [128, 128], BF16)
        c.tri = k.sb("tri", [128, 128], BF16); c.tri2 = k.sb("tri2", [128, 128], BF16)
        c.dconst = Dep()
        tmp = k.sb("ctmp", [128, 256], F32)
        k.dma("sp", c.ident[:], di["ident"][:, :], writes=[c.dconst])
        k.dma("sp", tmp[:, 0:128], di["tri"][:, :], writes=[c.dconst])
        k.dma("sp", tmp[:, 128:256], di["tri2"][:, :], writes=[c.dconst])
        k.op("dve", lambda e: e.tensor_copy(out=c.identb[:], in_=c.ident[:]), reads=[c.dconst], writes=[c.dconst])
        k.op("dve", lambda e: e.tensor_copy(out=c.tri[:], in_=tmp[:, 0:128]), reads=[c.dconst], writes=[c.dconst])
        k.op("dve", lambda e: e.tensor_copy(out=c.tri2[:], in_=tmp[:, 128:256]), reads=[c.dconst], writes=[c.dconst])
        c.ntri = k.sb("ntri", [128, 128], BF16); c.ntri2 = k.sb("ntri2", [128, 128], BF16)
        k.op("dve", lambda e: e.tensor_scalar(out=c.ntri[:], in0=tmp[:, 128:256], scalar1=-30000.0, scalar2=None, op0=ALU.mult),
             reads=[c.dconst], writes=[c.dconst])
        k.op("dve", lambda e: e.tensor_scalar(out=c.ntri2[:], in0=tmp[:, 0:128], scalar1=-30000.0, scalar2=None, op0=ALU.mult),
             reads=[c.dconst], writes=[c.dconst])
        c.outdeps = []
        c.dxs = [[Dep() for _ in range(NT)] for _ in range(nseq)]
        c.WG = k.sb("WG", [128, NT, 32], F32)
        c.dWG = Dep()
        c.epsb = k.sb("epsb", [128, 4], F32)
        k.op("pool", lambda e: e.memset(c.epsb[:, 0:1], EPS), writes=[c.dconst])
        k.op("pool", lambda e: e.memset(c.epsb[:, 1:2], -3.14159), writes=[c.dconst])
        k.op("pool", lambda e: e.memset(c.epsb[:, 2:4], -0.5), writes=[c.dconst])
        for s in range(nseq):
            layer0(c, s)
            if c.dbg:
                break
            with contextlib.ExitStack() as wst:
                W = moe_prefetch(c, 0, wst) if stop > 1 else None
                mixer_out(c, s, 0, "ab_w_o", c.x_d)
                if stop > 1:
                    moe(c, s, 0, (stop == 2), W)
            if stop <= 2:
                continue
            layer1(c, s)
            with contextlib.ExitStack() as wst:
                W = moe_prefetch(c, 1, wst) if stop > 3 else None
                mixer_out(c, s, 1, "c_w_o", c.xs_d)
                if stop > 3:
                    moe(c, s, 1, True, W)
        k.wait_all("sp", c.outdeps)
        c.nins = k.nins
    return nc, c


def bank(c, i):
    return c.ps[i], c.dps[i]


def transpose_to_XT(c, src, dsrc, i, extra32=None, dextra=None, banks=(0, 1)):
    k = c.k
    for half in range(2):
        ps, dp = bank(c, banks[half])
        for j in range(4):
            kc = half * 4 + j
            k.op("pe", lambda e: e.transpose(out=ps[:, j * 128:(j + 1) * 128], in_=src[:, kc * 128:(kc + 1) * 128],
                                             identity=c.ident[:]),
                 reads=[dsrc, c.dconst], writes=[dp], signal=(j == 3))
        psv = ps[:].rearrange("p (j t) -> p j t", j=4)
        k.op("act" if half == 0 else "dve",
             (lambda e: e.copy(out=c.XT[:, half * 4:half * 4 + 4, i * 128:(i + 1) * 128], in_=psv)) if half == 0 else
             (lambda e: e.tensor_copy(out=c.XT[:, half * 4:half * 4 + 4, i * 128:(i + 1) * 128], in_=psv)),
             reads=[dp], writes=[c.dXT[i]])
        if extra32 is not None:
            k.op("dve" if half == 0 else "act",
                 (lambda e: e.tensor_copy(out=extra32[:, half * 4:half * 4 + 4, :], in_=psv)) if half == 0 else
                 (lambda e: e.copy(out=extra32[:, half * 4:half * 4 + 4, :], in_=psv)),
                 reads=[dp], writes=[dextra])


def layer_norm_tile(c, R, dR, G, B, dGB, out, dout, st32, dst, par=0):
    k = c.k
    stats, mv, rstd = st32[par % len(st32)]
    dst = dst[par % len(dst)]
    for h in range(2):
        k.op("dve", lambda e: e.bn_stats(out=stats[:, h * 6:(h + 1) * 6], in_=R[:, h * 512:(h + 1) * 512]),
             reads=[dR], writes=[dst])
    k.op("dve", lambda e: e.bn_aggr(out=mv[:, 0:2], in_=stats[:, 0:12]), reads=[dst], writes=[dst])
    k.op("pool", lambda e: e.tensor_scalar(out=rstd[:, 0:1], in0=mv[:, 1:2], scalar1=EPS, scalar2=None, op0=ALU.add), reads=[dst], writes=[dst])
    k.op("pool", lambda e: e.tensor_tensor(out=rstd[:, 1:2], in0=rstd[:, 0:1], in1=c.epsb[:, 2:3], op=ALU.pow), reads=[dst, c.dconst], writes=[dst])
    k.op("dve", lambda e: e.tensor_scalar(out=R[:], in0=R[:], scalar1=mv[:, 0:1], scalar2=rstd[:, 1:2],
                                          op0=ALU.subtract, op1=ALU.mult), reads=[dR, dst], writes=[dR])
    k.op("dve", lambda e: e.tensor_tensor(out=R[:], in0=R[:], in1=G[:], op=ALU.mult), reads=[dR, dGB], writes=[dR])
    k.op("pool", lambda e: e.tensor_tensor(out=out[:, 0:384], in0=R[:, 0:384], in1=B[:, 0:384], op=ALU.add), reads=[dR, dGB], writes=[dout])
    k.op("dve", lambda e: e.tensor_tensor(out=out[:, 384:1024], in0=R[:, 384:1024], in1=B[:, 384:1024], op=ALU.add), reads=[dR, dGB], writes=[dout])


def mk_st32(c, ph, n=2):
    k = c.k
    return ([(k.sb("stats", [128, 12], F32, ph), k.sb("mv", [128, 2], F32, ph), k.sb("rstd", [128, 4], F32, ph)) for _ in range(n)],
            [Dep() for _ in range(n)])


def load_bf16(c, dst, src, dep):
    c.k.dma("pool", dst, src, writes=[dep])


def attention(c, ph, nheads, hg_size, qk_fn, kt_range_fn, ts_range_fn, mask_fn, v_fn, nv, fin_fn, tb, PT, dPT,
              acc_banks, s_banks, LA=4):
    k = c.k
    kts = kt_range_fn(tb)
    items = []
    for hg in range(nheads // hg_size):
        for kt in kts:
            for hi_ in range(hg_size):
                items.append((hg, kt, hi_, hg * hg_size + hi_))
    st = {}

    def front(n):
        hg, kt, hi_, h = items[n]
        lo, hi = ts_range_fn(tb, kt)
        c0, c1 = lo * 128, (hi + 1) * 128
        ps, dp = bank(c, s_banks[n % len(s_banks)])
        pt, dpt = PT[n % len(PT)], dPT[n % len(PT)]
        lhsT, rhs, rds = qk_fn(h, kt, tb * 512 + c0, tb * 512 + c1)
        masks = mask_fn(h, kt, lo, hi)
        k.op("pe", lambda e: e.matmul(ps[:, c0:c1], lhsT=lhsT, rhs=rhs, start=True, stop=(len(masks) == 0)), reads=rds, writes=[dp],
             signal=(len(masks) == 0))
        for mi_, (m0, m1, ml, mr_, mrd) in enumerate(masks):
            last = (mi_ == len(masks) - 1)
            k.op("pe", lambda e: e.matmul(ps[:, m0:m1], lhsT=ml, rhs=mr_, start=False, stop=last), reads=mrd, writes=[dp], signal=last)
        k.op("act", lambda e: e.activation(out=pt[:, c0:c1], in_=ps[:, c0:c1], func=AF.Exp), reads=[dp], writes=[dpt])
        st[n] = (pt, dpt, lo, hi)

    def finish(hg):
        for ts in range(4):
            pa, dpa = bank(c, acc_banks[ts])
            fin_fn(hg, ts, pa[:, 0:hg_size * nv].rearrange("p (h v) -> p h v", h=hg_size), dpa)

    def back(n):
        hg, kt, hi_, h = items[n]
        if n == 0 or items[n - 1][0] != hg:
            if n > 0:
                finish(items[n - 1][0])
            for ts in range(4):
                pa, dpa = bank(c, acc_banks[ts])
                k.op("dve", lambda e: e.memset(pa[:, 0:hg_size * nv], 0.0), writes=[dpa])
        pt, dpt, lo, hi = st.pop(n)
        vr, vrd = v_fn(h, kt)
        for ts in range(lo, hi + 1):
            pa, dpa = bank(c, acc_banks[ts])
            k.op("pe", lambda e: e.matmul(pa[:, hi_ * nv:(hi_ + 1) * nv], lhsT=pt[:, ts * 128:(ts + 1) * 128],
                                          rhs=vr, start=False, stop=False, skip_group_check=True),
                 reads=[dpt] + vrd, writes=[dpa], signal=(ts == hi))

    for n in range(min(LA, len(items))):
        front(n)
    for n in range(len(items)):
        back(n)
        if n + LA < len(items):
            front(n + LA)
    finish(items[-1][0])


def layer0(c, s):
    k, nc, di = c.k, c.nc, c.di
    with contextlib.ExitStack() as ph:
      p1 = ph.enter_context(contextlib.ExitStack())
      if True:
        XT, dXT = c.XT, c.dXT
        dW = Dep()
        KT = k.sb("KT", [128, 8, S], BF16, ph); dKT = [Dep() for _ in range(NT)]
        VA = k.sb("VA", [128, NT, 8, 65], BF16, ph); dVA = [Dep() for _ in range(NT)]
        CQT = k.sb("CQT", [128, 2, S], BF16, ph); dCQ = [Dep() for _ in range(NT)]
        RQ = k.sb("RQ", [128, NT], F32, ph)
        COS = k.sb("COS", [128, NT, 16], F32, ph)
        SIN = k.sb("SIN", [128, NT, 16], F32, ph)
        wuq = k.sb("wuq", [128, 2, 768], BF16, ph)
        xin = [k.sb("xin%d" % j, [128, D], F32, p1) for j in range(2)]
        dxin = [Dep(), Dep()]
        for i in range(2):
            k.dma("sp", xin[i][:], c.x_d[s, i * 128:(i + 1) * 128, :], writes=[dxin[i]])
        Win = k.sb("Win", [128, 8, 1440], BF16, p1)
        load_bf16(c, Win[:], di["ab_w_in"].rearrange("(kc p) n -> p kc n", p=128), dW)
        wuq32 = k.sb("wuq32", [128, 2, 768], F32, p1)
        wkv32 = k.sb("wkv32", [128, 1024], F32, p1)
        wkv = k.sb("wkv", [128, 1024], BF16, p1)
        qg = k.sb("qg", [128, 4], F32, p1)
        k.dma("sp", wuq32[:], di["w_uq"].rearrange("(kc p) n -> p kc n", p=128), writes=[dW])
        k.dma("sp", wkv32[:, 0:512], di["w_uk"][:, :], writes=[dW])
        k.dma("sp", wkv32[:, 512:1024], di["w_uv"][:, :], writes=[dW])
        k.dma("sp", qg[:, 0:2], di["qg2"][:, :], writes=[dW])
        k.dma("sp", qg[:, 2:3], di["kvg"][:, :], writes=[dW])
        for cc in range(2):
            k.op("dve", lambda e: e.tensor_scalar(out=wuq[:, cc, :], in0=wuq32[:, cc, :], scalar1=qg[:, cc:cc + 1],
                                                  scalar2=None, op0=ALU.mult), reads=[dW], writes=[dW])
        k.op("dve", lambda e: e.tensor_scalar(out=wkv[:], in0=wkv32[:], scalar1=qg[:, 2:3], scalar2=None, op0=ALU.mult),
             reads=[dW], writes=[dW])
        wsT32 = k.sb("wsT32", [128, 4, 128], F32, p1)
        wsT = k.sb("wsT", [128, 4, 128], BF16, p1)
        k.dma("sp", wsT32[:], di["gm_wsT"].rearrange("g s t -> s g t"), writes=[dW])
        tri32 = k.sb("tri32", [128, 128], F32, p1)
        k.dma("sp", tri32[:], di["tri"][:, :], writes=[dW])
        k.op("dve", lambda e: e.tensor_tensor(out=wsT[:], in0=wsT32[:], in1=tri32[:].unsqueeze(1).to_broadcast([128, 4, 128]),
                                              op=ALU.mult), reads=[dW], writes=[dW])
        bsB = k.sb("bsB", [128, 512], F32, p1)
        gmG = k.sb("gmG", [128, 512], F32, p1)
        gmB = k.sb("gmB", [128, 512], F32, p1)
        k.dma("sp", bsB[:], di["gm_bs"][0:1, :].partition_broadcast(128), writes=[dW])
        k.dma("sp", gmG[:], di["gm_ln_g"][0:1, :].partition_broadcast(128), writes=[dW])
        k.dma("sp", gmB[:], di["gm_ln_b"][0:1, :].partition_broadcast(128), writes=[dW])
        freq = k.sb("freq", [128, 16], F32, p1)
        k.dma("sp", freq[:], di["freq"][:, :], writes=[dW])
        posi = k.sb("posi", [128, NT], I32, p1)
        posf = k.sb("posf", [128, NT], F32, p1)
        k.dma("sp", posi[:], c.posT_d[:, s * NT:(s + 1) * NT], writes=[dW])
        k.op("dve", lambda e: e.tensor_copy(out=posf[:], in_=posi[:]), reads=[dW], writes=[dW])
        ang = k.sb("ang", [128, NT, 16], F32, p1)
        angi = k.sb("angi", [128, NT, 16], I32, p1)
        angf = k.sb("angf", [128, NT, 16], F32, p1)
        dR = Dep()
        for (tab, off) in ((SIN, 0.5), (COS, 0.75)):
            k.op("dve", lambda e: e.tensor_tensor(out=ang[:], in0=posf[:].unsqueeze(2).to_broadcast([128, NT, 16]),
                                                  in1=freq[:].unsqueeze(1).to_broadcast([128, NT, 16]), op=ALU.mult),
                 reads=[dW, dR], writes=[dR])
            k.op("dve", lambda e: e.tensor_scalar(out=ang[:], in0=ang[:], scalar1=off, scalar2=None, op0=ALU.add),
                 reads=[dR], writes=[dR])
            k.op("dve", lambda e: e.tensor_copy(out=angi[:], in_=ang[:]), reads=[dR], writes=[dR])
            k.op("dve", lambda e: e.tensor_copy(out=angf[:], in_=angi[:]), reads=[dR], writes=[dR])
            k.op("dve", lambda e: e.tensor_tensor(out=ang[:], in0=ang[:], in1=angf[:], op=ALU.subtract), reads=[dR], writes=[dR])
            k.op("dve", lambda e: e.tensor_scalar(out=angf[:], in0=ang[:], scalar1=0.0, scalar2=None, op0=ALU.is_lt),
                 reads=[dR], writes=[dR])
            k.op("dve", lambda e: e.tensor_tensor(out=ang[:], in0=ang[:], in1=angf[:], op=ALU.add), reads=[dR], writes=[dR])
            k.op("act", lambda e: e.activation(out=tab[:], in_=ang[:], func=AF.Sin, bias=c.epsb[:, 1:2], scale=6.28318),
                 reads=[dR, c.dconst], writes=[dR])
        k.op("pool", lambda e: e.memset(VA[:], 1.0), writes=dVA)
        for i in range(NT):
            b = i % 2
            if i >= 2:
                k.dma("sp", xin[b][:], c.x_d[s, i * 128:(i + 1) * 128, :], writes=[dxin[b]])
            transpose_to_XT(c, xin[b], dxin[b], i, banks=(0 + 2 * b, 1 + 2 * b))
        if c.dbg == "p0":
            dbg_exit(c, s); p1.close(); return
        gv = k.sb("gv", [128, 512], F32, p1); dgv = Dep()
        vn = k.sb("vn", [128, 512], BF16, p1); dvn = Dep()
        gu = k.sb("gu", [128, 512], F32, p1); dgu = Dep()
        stt = (k.sb("stats", [128, 12], F32, p1), k.sb("mv", [128, 2], F32, p1), k.sb("rstd", [128, 4], F32, p1))
        dst = Dep()
        sq = k.sb("sq", [128, 512], F32, p1); dsq = Dep()
        ssq = k.sb("ssq", [128, 4], F32, p1); dss = Dep()
        ckvT = k.sb("ckvT", [128, 128], BF16, p1); dck = Dep()
        Kt = k.sb("Kt", [128, 8, 96], BF16, p1); dKt = Dep()
        kr = k.sb("kr", [128, 64], F32, p1); dkr = Dep()
        tmpS = k.sb("tmpS", [128, 512], F32, p1); dtS = Dep()
        for i in range(NT):
            tok = slice(i * 128, (i + 1) * 128)
            pv, dpv = bank(c, 0); pc, dpc = bank(c, 1); pu, dpu = bank(c, 2); pct, dpct = bank(c, 3)
            for kc in range(8):
                k.op("pe", lambda e: e.matmul(pv[:, 0:512], lhsT=XT[:, kc, tok], rhs=Win[:, kc, 512:1024], start=(kc == 0), stop=(kc == 7)),
                     reads=[dXT[i], dW], writes=[dpv], signal=(kc == 7))
            for kc in range(8):
                k.op("pe", lambda e: e.matmul(pc[:, 0:416], lhsT=XT[:, kc, tok], rhs=Win[:, kc, 1024:1440], start=(kc == 0), stop=(kc == 7)),
                     reads=[dXT[i], dW], writes=[dpc], signal=(kc == 7))
            for cc in range(4):
                for kc in range(8):
                    k.op("pe", lambda e: e.matmul(pu[:, cc * 128:(cc + 1) * 128], lhsT=Win[:, kc, cc * 128:(cc + 1) * 128], rhs=XT[:, kc, tok],
                                                  start=(kc == 0), stop=(kc == 7)),
                         reads=[dXT[i], dW], writes=[dpu], signal=(kc == 7 and cc == 3))
            for cc in range(3):
                for kc in range(8):
                    k.op("pe", lambda e: e.matmul(pct[:, cc * 128:(cc + 1) * 128], lhsT=Win[:, kc, 1024 + cc * 128:1024 + (cc + 1) * 128],
                                                  rhs=XT[:, kc, tok], start=(kc == 0), stop=(kc == 7)),
                         reads=[dXT[i], dW], writes=[dpct], signal=(kc == 7 and cc == 2))
            k.op("act", lambda e: e.activation(out=gv[:], in_=pv[:, 0:512], func=AF.Gelu_apprx_tanh), reads=[dpv], writes=[dgv])
            k.op("dve", lambda e: e.bn_stats(out=stt[0][:, 0:6], in_=gv[:]), reads=[dgv], writes=[dst])
            k.op("dve", lambda e: e.bn_aggr(out=stt[1][:, 0:2], in_=stt[0][:, 0:6]), reads=[dst], writes=[dst])
            k.op("pool", lambda e: e.tensor_scalar(out=stt[2][:, 0:1], in0=stt[1][:, 1:2], scalar1=EPS, scalar2=None, op0=ALU.add), reads=[dst], writes=[dst])
            k.op("pool", lambda e: e.tensor_tensor(out=stt[2][:, 1:2], in0=stt[2][:, 0:1], in1=c.epsb[:, 2:3], op=ALU.pow), reads=[dst, c.dconst], writes=[dst])
            k.op("dve", lambda e: e.tensor_scalar(out=gv[:], in0=gv[:], scalar1=stt[1][:, 0:1], scalar2=stt[2][:, 1:2],
                                                  op0=ALU.subtract, op1=ALU.mult), reads=[dgv, dst], writes=[dgv])
            k.op("pool", lambda e: e.tensor_tensor(out=gv[:], in0=gv[:], in1=gmG[:], op=ALU.mult), reads=[dgv, dW], writes=[dgv])
            k.op("pool", lambda e: e.tensor_tensor(out=vn[:], in0=gv[:], in1=gmB[:], op=ALU.add), reads=[dgv, dW], writes=[dvn])
            k.op("act", lambda e: e.activation(out=gu[:], in_=pu[:, 0:512], func=AF.Gelu_apprx_tanh), reads=[dpu], writes=[dgu])
            ps_, dps_ = bank(c, 4)
            for g in range(4):
                k.op("pe", lambda e: e.matmul(ps_[:, g * 128:(g + 1) * 128], lhsT=vn[:, g * 128:(g + 1) * 128], rhs=wsT[:, g, :],
                                              start=True, stop=True), reads=[dvn, dW], writes=[dps_], signal=(g == 3))
            k.op("dve", lambda e: e.tensor_tensor(out=tmpS[:], in0=ps_[:, 0:512], in1=bsB[:], op=ALU.add), reads=[dps_, dW], writes=[dtS])
            k.op("pool", lambda e: e.memset(ssq[:, 0:2], 0.0), writes=[dss])
            k.op("act", lambda e: e.activation(out=sq[:, 0:256], in_=pc[:, 0:256], func=AF.Square, accum_out=ssq[:, 0:1]),
                 reads=[dpc], writes=[dsq, dss])
            k.op("act", lambda e: e.activation(out=sq[:, 256:384], in_=pc[:, 256:384], func=AF.Square, accum_out=ssq[:, 1:2]),
                 reads=[dpc], writes=[dsq, dss])
            k.op("pool", lambda e: e.tensor_scalar(out=ssq[:, 2:3], in0=ssq[:, 0:1], scalar1=1.0 / 256, scalar2=EPS, op0=ALU.mult, op1=ALU.add),
                 reads=[dss], writes=[dss])
            k.op("pool", lambda e: e.tensor_scalar(out=ssq[:, 3:4], in0=ssq[:, 1:2], scalar1=1.0 / 128, scalar2=EPS, op0=ALU.mult, op1=ALU.add),
                 reads=[dss], writes=[dss])
            k.op("pool", lambda e: e.tensor_tensor(out=ssq[:, 0:2], in0=ssq[:, 2:4], in1=c.epsb[:, 2:4], op=ALU.pow), reads=[dss, c.dconst], writes=[dss])
            k.op("dve", lambda e: e.tensor_scalar(out=RQ[:, i:i + 1], in0=ssq[:, 0:1], scalar1=96.0 ** -0.5, scalar2=None, op0=ALU.mult),
                 reads=[dss], writes=[dCQ[i]])
            k.op("act", lambda e: e.copy(out=kr[:, 0:32], in_=pc[:, 384:416]), reads=[dpc], writes=[dkr])
            rope(c, kr[:, 0:32].rearrange("p (o r) -> p o r", o=1), Kt[:, 0:1, 64:96], COS[:, i:i + 1, :], SIN[:, i:i + 1, :],
                 kr[:, 32:64].rearrange("p (o r) -> p o r", o=1), [dkr, dR], dkr, dKt, 1)
            for h in range(1, 8):
                k.op("pool", lambda e: e.tensor_copy(out=Kt[:, h, 64:96], in_=Kt[:, 0, 64:96]), reads=[dKt], writes=[dKt])
            k.op("dve", lambda e: e.tensor_tensor(out=XT[:, 0:4, tok], in0=tmpS[:].rearrange("p (g t) -> p g t", g=4),
                                                  in1=gu[:].rearrange("p (g t) -> p g t", g=4), op=ALU.mult),
                 reads=[dtS, dgu], writes=[dXT[i]])
            k.op("act", lambda e: e.copy(out=CQT[:, :, tok], in_=pct[:, 0:256].rearrange("p (c t) -> p c t", c=2)),
                 reads=[dpct], writes=[dCQ[i]])
            k.op("act", lambda e: e.copy(out=ckvT[:], in_=pct[:, 256:384]), reads=[dpct], writes=[dck])
            pk, dpk = bank(c, 5); pvv, dpvv = bank(c, 6)
            k.op("pe", lambda e: e.matmul(pk[:, 0:512], lhsT=ckvT[:], rhs=wkv[:, 0:512], start=True, stop=True), reads=[dck, dW], writes=[dpk])
            k.op("pe", lambda e: e.matmul(pvv[:, 0:512], lhsT=ckvT[:], rhs=wkv[:, 512:1024], start=True, stop=True), reads=[dck, dW], writes=[dpvv])
            k.op("dve", lambda e: e.tensor_scalar(out=Kt[:, :, 0:64], in0=pk[:, 0:512].rearrange("p (h d) -> p h d", h=8),
                                                  scalar1=ssq[:, 1:2], scalar2=None, op0=ALU.mult), reads=[dpk, dss], writes=[dKt])
            k.op("act", lambda e: e.activation(out=VA[:, i, :, 0:64], in_=pvv[:, 0:512].rearrange("p (h d) -> p h d", h=8),
                                               func=AF.Copy, scale=ssq[:, 1:2]), reads=[dpvv, dss], writes=[dVA[i]])
            ptr, dptr = bank(c, 7)
            ptrb = ptr[:].bitcast(BF16)
            for h in range(8):
                k.op("pe", lambda e: e.transpose(out=ptrb[0:96, h * 128:(h + 1) * 128], in_=Kt[:, h, :], identity=c.identb[:]),
                     reads=[dKt, c.dconst], writes=[dptr], signal=(h == 7))
            k.op("act", lambda e: e.copy(out=KT[0:96, :, tok], in_=ptrb[0:96, :].rearrange("p (h t) -> p h t", h=8)),
                 reads=[dptr], writes=[dKT[i]])
        if c.dbg == "p1":
            dbg_exit(c, s); p1.close(); return
        k.barrier()
        p1.close()
        QT = [k.sb("QT%d" % j, [128, 8, 512], BF16, ph) for j in range(2)]
        dQT = [Dep(), Dep()]
        Qt = k.sb("Qt", [128, 8, 96], BF16, ph); dQt = Dep()
        qr = k.sb("qr", [128, 8, 32], F32, ph); dqr = Dep()
        qtmp = k.sb("qtmp", [128, 8, 32], F32, ph)
        PT = [k.sb("PT%d" % j, [128, 512], BF16, ph) for j in range(6)]
        dPT = [Dep() for _ in range(6)]
        Ot = k.sb("Ot", [128, 4, 512], BF16, ph); dOt = [Dep() for _ in range(4)]
        rden = k.sb("rden", [128, 8], F32, ph); drd = Dep()
        if c.dbg == "p2z":
            dbg_exit(c, s); return
        def qprep(tb):
            qb = tb % 2
            for ts in range(4):
                i = tb * 4 + ts
                tok = slice(i * 128, (i + 1) * 128)
                pq = [bank(c, 4), bank(c, 5)]
                for hf in range(2):
                    for cc in range(2):
                        k.op("pe", lambda e: e.matmul(pq[hf][0][:, 0:384], lhsT=CQT[:, cc, tok], rhs=wuq[:, cc, hf * 384:(hf + 1) * 384],
                                                      start=(cc == 0), stop=(cc == 1)), reads=[dCQ[i], dW], writes=[pq[hf][1]],
                             signal=(cc == 1))
                for hf in range(2):
                    pqv = pq[hf][0][:, 0:384].rearrange("p (h d) -> p h d", h=4)
                    k.op("dve", lambda e: e.tensor_scalar(out=Qt[:, hf * 4:hf * 4 + 4, 0:64], in0=pqv[:, :, 0:64], scalar1=RQ[:, i:i + 1],
                                                          scalar2=None, op0=ALU.mult), reads=[pq[hf][1], dCQ[i]], writes=[dQt])
                    k.op("dve", lambda e: e.tensor_scalar(out=qr[:, hf * 4:hf * 4 + 4, :], in0=pqv[:, :, 64:96], scalar1=RQ[:, i:i + 1],
                                                          scalar2=None, op0=ALU.mult), reads=[pq[hf][1], dCQ[i]], writes=[dqr])
                rope(c, qr[:], Qt[:, :, 64:96], COS[:, i:i + 1, :], SIN[:, i:i + 1, :], qtmp[:], [dqr, dR], dqr, dQt, 8)
                ptr, dptr = bank(c, 6 + (ts % 2))
                ptrb = ptr[:].bitcast(BF16)
                for h in range(8):
                    k.op("pe", lambda e: e.transpose(out=ptrb[0:96, h * 128:(h + 1) * 128], in_=Qt[:, h, :], identity=c.identb[:]),
                         reads=[dQt, c.dconst], writes=[dptr], signal=(h == 7))
                k.op("act", lambda e: e.copy(out=QT[qb][0:96, :, ts * 128:(ts + 1) * 128],
                                             in_=ptrb[0:96, :].rearrange("p (h t) -> p h t", h=8)), reads=[dptr], writes=[dQT[qb]])

        qprep(0)
        for tb in range(4):
            qb = tb % 2
            if tb + 1 < 4:
                qprep(tb + 1)

            def qk_fn(h, kt, t0, t1):
                return (KT[0:96, h, kt * 128:(kt + 1) * 128], QT[qb][0:96, h, t0 - tb * 512:t1 - tb * 512], [dKT[kt], dQT[qb]])

            def kt_range(tb_):
                return list(range(0, 4 * tb_ + 4))

            def ts_range(tb_, kt):
                return (max(0, kt - 4 * tb_), 3)

            def mask_fn(h, kt, lo, hi):
                if kt >= 4 * tb:
                    return [(lo * 128, lo * 128 + 128, c.identb[:], c.ntri[:], [c.dconst])]
                return []

            def v_fn(h, kt):
                return (VA[:, kt, h, :], [dVA[kt]])

            def fin(hg, ts, acc, dacc):
                k.op("dve", lambda e: e.reciprocal(out=rden[:, 0:4], in_=acc[:, :, 64]), reads=[dacc], writes=[drd])
                k.op("dve", lambda e: e.tensor_tensor(out=Ot[:, ts, hg * 256:(hg + 1) * 256].rearrange("p (h d) -> p h d", h=4),
                                                      in0=acc[:, :, 0:64], in1=rden[:, 0:4].unsqueeze(2).to_broadcast([128, 4, 64]),
                                                      op=ALU.mult), reads=[dacc, drd], writes=[dOt[ts]])

            attention(c, ph, 8, 4, qk_fn, kt_range, ts_range, mask_fn, v_fn, 65, fin, tb, PT, dPT,
                      acc_banks=(0, 1, 2, 3), s_banks=(4, 5, 6, 7))
            for ts in range(4):
                i = tb * 4 + ts
                ptr, dptr = bank(c, 6 + (ts % 2))
                ptrb = ptr[:].bitcast(BF16)
                for cc in range(4):
                    k.op("pe", lambda e: e.transpose(out=ptrb[:, cc * 128:(cc + 1) * 128], in_=Ot[:, ts, cc * 128:(cc + 1) * 128],
                                                     identity=c.identb[:]), reads=[dOt[ts], c.dconst], writes=[dptr], signal=(cc == 3))
                k.op("act", lambda e: e.copy(out=XT[:, 4:8, i * 128:(i + 1) * 128], in_=ptrb[:, 0:512].rearrange("p (c t) -> p c t", c=4)),
                     reads=[dptr], writes=[dXT[i]])
        if c.dbg == "p2":
            return dbg_exit(c, s)
        k.barrier()


def dbg_exit(c, s):
    k = c.k
    k.barrier()
    od = Dep()
    k.dma("sp", c.out_d[s, 0:128, :], c.x_d[s, 0:128, :], writes=[od])
    c.outdeps.append(od)
    k.wait_all("sp", c.outdeps)


def rope(c, x, out, cos, sin, tmp, rdeps, dtmp, dout, nh):
    k = c.k
    cb = cos.to_broadcast([128, nh, 16])
    sb_ = sin.to_broadcast([128, nh, 16])
    x1, x2 = x[:, :, 0:16], x[:, :, 16:32]
    k.op("dve", lambda e: e.tensor_tensor(out=tmp[:, :, 0:16], in0=x1, in1=cb, op=ALU.mult), reads=rdeps, writes=[dtmp])
    k.op("dve", lambda e: e.tensor_tensor(out=tmp[:, :, 16:32], in0=x2, in1=sb_, op=ALU.mult), reads=rdeps, writes=[dtmp])
    k.op("dve", lambda e: e.tensor_tensor(out=out[:, :, 0:16], in0=tmp[:, :, 0:16], in1=tmp[:, :, 16:32], op=ALU.subtract),
         reads=[dtmp], writes=[dout])
    k.op("dve", lambda e: e.tensor_tensor(out=tmp[:, :, 0:16], in0=x1, in1=sb_, op=ALU.mult), reads=rdeps, writes=[dtmp])
    k.op("dve", lambda e: e.tensor_tensor(out=tmp[:, :, 16:32], in0=x2, in1=cb, op=ALU.mult), reads=rdeps, writes=[dtmp])
    k.op("dve", lambda e: e.tensor_tensor(out=out[:, :, 16:32], in0=tmp[:, :, 0:16], in1=tmp[:, :, 16:32], op=ALU.add),
         reads=[dtmp], writes=[dout])


def mixer_out(c, s, layer, wo_name, xsrc_d):
    k, nc, di = c.k, c.nc, c.di
    XT, dXT = c.XT, c.dXT
    with contextlib.ExitStack() as ph:
        dW = Dep()
        Wo = k.sb("Wo", [128, 8, D], BF16, ph)
        load_bf16(c, Wo[:], di[wo_name].rearrange("(kc p) n -> p kc n", p=128), dW)
        G = k.sb("lnG", [128, D], F32, ph); B = k.sb("lnB", [128, D], F32, ph)
        k.dma("sp", G[:], di["ln1_g"][layer:layer + 1, :].partition_broadcast(128), writes=[dW])
        k.dma("sp", B[:], di["ln1_b"][layer:layer + 1, :].partition_broadcast(128), writes=[dW])
        Wr = k.sb("Wr", [128, 8, 36], F32, ph)
        k.dma("sp", Wr[:, :, 0:4], di["moe_w_rg"][layer].rearrange("(kc p) n -> p kc n", p=128), writes=[dW])
        k.dma("sp", Wr[:, :, 4:36], di["moe_w_re"][layer].rearrange("(kc p) n -> p kc n", p=128), writes=[dW])
        Br = k.sb("Br", [128, 36], F32, ph)
        k.dma("sp", Br[:, 0:4], di["moe_b_rg"][layer:layer + 1, :].partition_broadcast(128), writes=[dW])
        k.dma("sp", Br[:, 4:36], di["moe_b_re"][layer:layer + 1, :].partition_broadcast(128), writes=[dW])
        xin = [k.sb("xin%d" % j, [128, D], F32, ph) for j in range(2)]
        dxin = [Dep(), Dep()]
        R = [k.sb("R%d" % j, [128, D], F32, ph) for j in range(2)]
        dRr = [Dep(), Dep()]
        X1 = [k.sb("X1%d" % j, [128, D], F32, ph) for j in range(2)]
        dX1 = [Dep(), Dep()]
        xT32 = [k.sb("xT32%d" % j, [128, 8, 128], F32, ph) for j in range(2)]
        dxT32 = [Dep(), Dep()]
        st32, dst = mk_st32(c, ph)
        RT = k.sb("RT", [128, NT, 128], F32, ph); dRT = Dep()
        dL = [Dep() for _ in range(NT)]
        def stageApe(i):
            b = i % 2
            tok = slice(i * 128, (i + 1) * 128)
            k.dma("sp", xin[b][:], xsrc_d[s, tok, :], reads=[c.dxs[s][i]], writes=[dxin[b]])
            pm = [bank(c, 0 + 2 * (i % 3)), bank(c, 1 + 2 * (i % 3))]
            for hf in range(2):
                for kc in range(8):
                    k.op("pe", lambda e: e.matmul(pm[hf][0][:, 0:512], lhsT=XT[:, kc, tok], rhs=Wo[:, kc, hf * 512:(hf + 1) * 512],
                                                  start=(kc == 0), stop=(kc == 7)), reads=[dXT[i], dW], writes=[pm[hf][1]], signal=(kc == 7))

        def stageAdve(i):
            b = i % 2
            pm = [bank(c, 0 + 2 * (i % 3)), bank(c, 1 + 2 * (i % 3))]
            for hf in range(2):
                k.op("dve", lambda e: e.scalar_tensor_tensor(out=R[b][:, hf * 512:(hf + 1) * 512], in0=xin[b][:, hf * 512:(hf + 1) * 512],
                                                             scalar=ALPHA, in1=pm[hf][0][:, 0:512], op0=ALU.mult, op1=ALU.add),
                     reads=[dxin[b], pm[hf][1]], writes=[dRr[b]])
            layer_norm_tile(c, R[b], dRr[b], G, B, dW, X1[b][:], dX1[b], st32, dst, par=i)

        def stageB(i):
            b = i % 2
            tok = slice(i * 128, (i + 1) * 128)
            k.dma("sp", c.xs_d[s, tok, :], X1[b][:], reads=[dX1[b]], writes=[c.dxs[s][i]])
            if c.stop == 2 * layer + 1:
                od = Dep()
                k.dma("sp", c.out_d[s, tok, :], X1[b][:], reads=[dX1[b]], writes=[od])
                c.outdeps.append(od)
            transpose_to_XT(c, X1[b], dX1[b], i, extra32=xT32[b], dextra=dxT32[b], banks=(6, 6))
            pl, dpl = bank(c, 7)
            for kc in range(8):
                k.op("pe", lambda e: e.matmul(pl[:, 0:36], lhsT=xT32[b][:, kc, :], rhs=Wr[:, kc, :], start=(kc == 0), stop=(kc == 7)),
                     reads=[dxT32[b], dW], writes=[dpl], signal=(kc == 7))
            k.op("act", lambda e: e.copy(out=RT[:, i, 0:36], in_=pl[:, 0:36]), reads=[dpl], writes=[dL[i]])

        stageApe(0)
        stageApe(1)
        stageAdve(0)
        for i in range(NT):
            if i + 2 < NT:
                stageApe(i + 2)
            if i + 1 < NT:
                stageAdve(i + 1)
            stageB(i)
        router_all(c, RT, dRT, dL, Br, dW)
        k.barrier()


def router_all(c, RT, dRT, dL, Br, dBr):
    k = c.k
    L = RT[:, :, 0:36]
    f = lambda a, b: RT[:, :, a:b]
    gm, ngs, gw = f(36, 37), f(37, 38), f(38, 39)
    oh, ex = f(40, 44), f(44, 48)
    esel, tmp8, eq1, eq2 = f(48, 56), f(56, 64), f(64, 72), f(72, 80)
    m1, m2, dlt, w1, w2 = f(80, 81), f(81, 82), f(82, 83), f(83, 84), f(84, 85)
    wg = f(88, 120)
    bc = lambda ap, n: ap.to_broadcast([128, NT, n])
    first = [True]

    def V(fn, eng="dve"):
        rd = [dRT] + (list(dL) if first[0] else [])
        first[0] = False
        k.op(eng, fn, reads=rd, writes=[dRT])
    k.op("dve", lambda e: e.tensor_tensor(out=L, in0=L, in1=Br[:].unsqueeze(1).to_broadcast([128, NT, 36]), op=ALU.add),
         reads=[dRT, dBr] + list(dL), writes=[dRT])
    first[0] = False
    V(lambda e: e.tensor_reduce(out=gm, in_=L[:, :, 0:4], axis=AX.X, op=ALU.max))
    V(lambda e: e.tensor_tensor(out=oh, in0=L[:, :, 0:4], in1=bc(gm, 4), op=ALU.is_ge))
    V(lambda e: e.tensor_tensor(out=ex, in0=L[:, :, 0:4], in1=bc(gm, 4), op=ALU.subtract))
    V(lambda e: e.activation(out=ex, in_=ex, func=AF.Exp), "act")
    V(lambda e: e.tensor_reduce(out=ngs, in_=ex, axis=AX.X, op=ALU.add))
    V(lambda e: e.reciprocal(out=gw, in_=ngs))
    V(lambda e: e.tensor_tensor(out=esel, in0=L[:, :, 4:12], in1=bc(oh[:, :, 0:1], 8), op=ALU.mult))
    for g in range(1, 4):
        V(lambda e: e.tensor_tensor(out=tmp8, in0=L[:, :, 4 + 8 * g:12 + 8 * g], in1=bc(oh[:, :, g:g + 1], 8), op=ALU.mult))
        V(lambda e: e.tensor_tensor(out=esel, in0=esel, in1=tmp8, op=ALU.add))
    V(lambda e: e.tensor_reduce(out=m1, in_=esel, axis=AX.X, op=ALU.max))
    V(lambda e: e.tensor_tensor(out=eq1, in0=esel, in1=bc(m1, 8), op=ALU.is_ge))
    V(lambda e: e.tensor_scalar(out=tmp8, in0=eq1, scalar1=-1e30, scalar2=None, op0=ALU.mult))
    V(lambda e: e.tensor_tensor(out=tmp8, in0=tmp8, in1=esel, op=ALU.add))
    V(lambda e: e.tensor_reduce(out=m2, in_=tmp8, axis=AX.X, op=ALU.max))
    V(lambda e: e.tensor_tensor(out=eq2, in0=tmp8, in1=bc(m2, 8), op=ALU.is_ge))
    V(lambda e: e.tensor_tensor(out=dlt, in0=m2, in1=m1, op=ALU.subtract))
    V(lambda e: e.activation(out=dlt, in_=dlt, func=AF.Exp), "act")
    V(lambda e: e.tensor_scalar(out=dlt, in0=dlt, scalar1=1.0, scalar2=None, op0=ALU.add))
    V(lambda e: e.reciprocal(out=w1, in_=dlt))
    V(lambda e: e.tensor_scalar(out=w2, in0=w1, scalar1=-1.0, scalar2=1.0, op0=ALU.mult, op1=ALU.add))
    V(lambda e: e.tensor_tensor(out=w1, in0=w1, in1=gw, op=ALU.mult))
    V(lambda e: e.tensor_tensor(out=w2, in0=w2, in1=gw, op=ALU.mult))
    V(lambda e: e.tensor_tensor(out=eq1, in0=eq1, in1=bc(w1, 8), op=ALU.mult))
    V(lambda e: e.tensor_tensor(out=eq2, in0=eq2, in1=bc(w2, 8), op=ALU.mult))
    V(lambda e: e.tensor_tensor(out=eq1, in0=eq1, in1=eq2, op=ALU.add))
    for g in range(4):
        V(lambda e: e.tensor_tensor(out=wg[:, :, g * 8:(g + 1) * 8], in0=eq1, in1=bc(oh[:, :, g:g + 1], 8), op=ALU.mult))
    k.op("dve", lambda e: e.tensor_copy(out=c.WG[:], in_=wg), reads=[dRT], writes=[c.dWG])


def router_tile(c, i, xT32, dxT32, Wr, Br, dW, rt, drt, wg):
    k = c.k
    pl, dpl = bank(c, 6)
    for kc in range(8):
        k.op("pe", lambda e: e.matmul(pl[:, 0:36], lhsT=xT32[:, kc, :], rhs=Wr[:, kc, :], start=(kc == 0), stop=(kc == 7)),
             reads=[dxT32, dW], writes=[dpl], signal=(kc == 7))
    L = rt[:, 0:36]
    k.op("dve", lambda e: e.tensor_tensor(out=L, in0=pl[:, 0:36], in1=Br[:], op=ALU.add), reads=[dpl, dW], writes=[drt])
    gm = rt[:, 36:37]; oh = rt[:, 40:44]; ex = rt[:, 44:48]; gs = rt[:, 48:49]; gw = rt[:, 49:50]
    esel = rt[:, 52:60]; top8 = rt[:, 60:68]; dlt = rt[:, 68:69]; w1 = rt[:, 69:70]; w2 = rt[:, 70:71]
    m1 = rt[:, 72:80]; m2 = rt[:, 80:88]; we = rt[:, 88:96]; ngm = rt[:, 37:38]
    V = lambda fn, **kw: k.op("dve", fn, reads=[drt], writes=[drt])
    V(lambda e: e.tensor_reduce(out=gm, in_=L[:, 0:4], axis=AX.X, op=ALU.max))
    V(lambda e: e.tensor_scalar(out=oh, in0=L[:, 0:4], scalar1=gm, scalar2=None, op0=ALU.is_ge))
    V(lambda e: e.tensor_scalar(out=ngm, in0=gm, scalar1=-1.0, scalar2=None, op0=ALU.mult))
    k.op("pool", lambda e: e.memset(gs, 0.0), reads=[drt], writes=[drt])
    k.op("act", lambda e: e.activation(out=ex, in_=L[:, 0:4], func=AF.Exp, bias=ngm, scale=1.0, accum_out=gs), reads=[drt], writes=[drt])
    V(lambda e: e.reciprocal(out=gw, in_=gs))
    V(lambda e: e.tensor_scalar(out=esel, in0=L[:, 4:12], scalar1=oh[:, 0:1], scalar2=None, op0=ALU.mult))
    for g in range(1, 4):
        V(lambda e: e.scalar_tensor_tensor(out=esel, in0=L[:, 4 + 8 * g:12 + 8 * g], scalar=oh[:, g:g + 1], in1=esel,
                                           op0=ALU.mult, op1=ALU.add))
    V(lambda e: e.max(out=top8, in_=esel))
    V(lambda e: e.tensor_tensor(out=dlt, in0=top8[:, 1:2], in1=top8[:, 0:1], op=ALU.subtract))
    k.op("act", lambda e: e.activation(out=dlt, in_=dlt, func=AF.Exp), reads=[drt], writes=[drt])
    V(lambda e: e.tensor_scalar(out=dlt, in0=dlt, scalar1=1.0, scalar2=None, op0=ALU.add))
    V(lambda e: e.reciprocal(out=w1, in_=dlt))
    V(lambda e: e.tensor_scalar(out=w2, in0=w1, scalar1=-1.0, scalar2=1.0, op0=ALU.mult, op1=ALU.add))
    V(lambda e: e.tensor_tensor(out=w1, in0=w1, in1=gw, op=ALU.mult))
    V(lambda e: e.tensor_tensor(out=w2, in0=w2, in1=gw, op=ALU.mult))
    V(lambda e: e.tensor_scalar(out=m1, in0=esel, scalar1=top8[:, 0:1], scalar2=w1, op0=ALU.is_ge, op1=ALU.mult))
    V(lambda e: e.tensor_scalar(out=m2, in0=esel, scalar1=top8[:, 1:2], scalar2=w2, op0=ALU.is_equal, op1=ALU.mult))
    V(lambda e: e.tensor_tensor(out=we, in0=m1, in1=m2, op=ALU.add))
    for g in range(4):
        k.op("dve", lambda e: e.tensor_scalar(out=wg[:, g * 8:(g + 1) * 8], in0=we, scalar1=oh[:, g:g + 1], scalar2=None, op0=ALU.mult),
             reads=[drt], writes=[drt])
    pw, dpw = bank(c, 7)
    k.op("pe", lambda e: e.transpose(out=pw[0:32, 0:128], in_=wg[:, 0:32], identity=c.ident[:]), reads=[drt, c.dconst], writes=[dpw])
    k.op("act", lambda e: e.copy(out=c.WGT[:, i * 128:(i + 1) * 128], in_=pw[0:32, 0:128]), reads=[dpw], writes=[c.dWGT[i // 4]])


def moe_prefetch(c, layer, st):
    k, di = c.k, c.di
    NW = 3
    Wg = [k.sb("Wg%d" % j, [128, 8, 256], BF16, st) for j in range(NW)]
    Wu = [k.sb("Wu%d" % j, [128, 8, 256], BF16, st) for j in range(NW)]
    Wd = [k.sb("Wd%d" % j, [128, 2, D], BF16, st) for j in range(NW)]
    dWe = [Dep() for _ in range(NW)]

    def loadw(e_):
        j = e_ % NW
        load_bf16(c, Wg[j][:], di["moe_w_gate"][layer, e_].rearrange("(kc p) n -> p kc n", p=128), dWe[j])
        load_bf16(c, Wu[j][:], di["moe_w_up"][layer, e_].rearrange("(kc p) n -> p kc n", p=128), dWe[j])
        load_bf16(c, Wd[j][:], di["moe_w_down"][layer, e_].rearrange("(kc p) n -> p kc n", p=128), dWe[j])
    for e_ in range(NW):
        loadw(e_)
    return (NW, Wg, Wu, Wd, dWe, loadw)


def moe(c, s, layer, final, W):
    k, nc, di = c.k, c.nc, c.di
    XT, dXT = c.XT, c.dXT
    NW, Wg, Wu, Wd, dWe, loadw = W
    with contextlib.ExitStack() as ph:
        Y = k.sb("Y", [128, NT, D], F32, ph); dY = [Dep() for _ in range(NT)]
        for i in range(NT):
            k.dma("sp", Y[:, i, :], c.xs_d[s, i * 128:(i + 1) * 128, :], reads=[c.dxs[s][i]], writes=[dY[i]])
            if i % 2 == 0:
                k.op("act", lambda e: e.activation(out=Y[:, i, :], in_=Y[:, i, :], func=AF.Copy, scale=ALPHA), reads=[dY[i]], writes=[dY[i]])
            else:
                k.op("dve", lambda e: e.tensor_scalar(out=Y[:, i, :], in0=Y[:, i, :], scalar1=ALPHA, scalar2=None, op0=ALU.mult),
                     reads=[dY[i]], writes=[dY[i]])
        sg = [k.sb("sg%d" % j, [128, 512], F32, ph) for j in range(2)]; dsg = [Dep(), Dep()]
        wb = [k.sb("wb%d" % j, [128, 512], F32, ph) for j in range(2)]; dwb = [Dep(), Dep()]
        act = [k.sb("act%d" % j, [128, 2, 512], BF16, ph) for j in range(2)]; dact = [Dep(), Dep()]
        items = [(e_, sbk) for e_ in range(32) for sbk in range(4)]
        dn = [4, 5, 6, 7]
        dni = [0]

        def front(n, fs=(0, 1)):
            e_, sbk = items[n]
            j = e_ % NW
            a = n % 2
            t0 = sbk * 512
            for f in fs:
                pg, dpg = bank(c, 0 + f); pu, dpu = bank(c, 2 + f)
                for kc in range(8):
                    k.op("pe", lambda e: e.matmul(pg[:, 0:512], lhsT=Wg[j][:, kc, f * 128:(f + 1) * 128], rhs=XT[:, kc, t0:t0 + 512],
                                                  start=(kc == 0), stop=(kc == 7)),
                         reads=[dWe[j]] + dXT[sbk * 4:sbk * 4 + 4], writes=[dpg], signal=(kc == 7))
                for kc in range(8):
                    k.op("pe", lambda e: e.matmul(pu[:, 0:512], lhsT=Wu[j][:, kc, f * 128:(f + 1) * 128], rhs=XT[:, kc, t0:t0 + 512],
                                                  start=(kc == 0), stop=(kc == 7)),
                         reads=[dWe[j]] + dXT[sbk * 4:sbk * 4 + 4], writes=[dpu], signal=(kc == 7))
                k.op("act", lambda e: e.activation(out=sg[f][:], in_=pg[:, 0:512], func=AF.Silu), reads=[dpg], writes=[dsg[f]])
                k.op("dve", lambda e: e.tensor_tensor(out=act[a][:, f, :], in0=sg[f][:], in1=pu[:, 0:512], op=ALU.mult),
                     reads=[dsg[f], dpu], writes=[dact[a]])

        def back(n, tts=(0, 1, 2, 3)):
            e_, sbk = items[n]
            j = e_ % NW
            a = n % 2
            for tt in tts:
                i = sbk * 4 + tt
                for hf in range(2):
                    po, dpo = bank(c, dn[dni[0] % 4]); dni[0] += 1
                    for f in range(2):
                        k.op("pe", lambda e: e.matmul(po[:, 0:512], lhsT=act[a][:, f, tt * 128:(tt + 1) * 128],
                                                      rhs=Wd[j][:, f, hf * 512:(hf + 1) * 512], start=(f == 0), stop=(f == 1)),
                             reads=[dact[a], dWe[j]], writes=[dpo], signal=(f == 1))
                    k.op("dve", lambda e: e.scalar_tensor_tensor(out=Y[:, i, hf * 512:(hf + 1) * 512], in0=po[:, 0:512],
                                                                 scalar=c.WG[:, i, e_:e_ + 1], in1=Y[:, i, hf * 512:(hf + 1) * 512],
                                                                 op0=ALU.mult, op1=ALU.add),
                         reads=[dpo, dY[i], c.dWG], writes=[dY[i]])
            if sbk == 3 and e_ + 3 < 32 and 3 in tts:
                loadw(e_ + 3)

        front(0)
        for n in range(len(items)):
            if n + 1 < len(items):
                front(n + 1, (0,))
            back(n, (0, 1))
            if n + 1 < len(items):
                front(n + 1, (1,))
            back(n, (2, 3))
        dW = Dep()
        G = k.sb("lnG", [128, D], F32, ph); B = k.sb("lnB", [128, D], F32, ph)
        k.dma("sp", G[:], di["ln2_g"][layer:layer + 1, :].partition_broadcast(128), writes=[dW])
        k.dma("sp", B[:], di["ln2_b"][layer:layer + 1, :].partition_broadcast(128), writes=[dW])
        st32, dst = mk_st32(c, ph)
        def lnA(i):
            Yi = Y[:, i, :]
            layer_norm_tile(c, _V(Yi), dY[i], G, B, dW, Yi, dY[i], st32, dst, par=i)

        def lnB(i):
            tok = slice(i * 128, (i + 1) * 128)
            Yi = Y[:, i, :]
            if final:
                od = Dep()
                k.dma("sp", c.out_d[s, tok, :], Yi, reads=[dY[i]], writes=[od])
                c.outdeps.append(od)
            else:
                k.dma("sp", c.xs_d[s, tok, :], Yi, reads=[dY[i]], writes=[c.dxs[s][i]])
                transpose_to_XT(c, _V(Yi), dY[i], i, banks=(0 + 2 * (i % 2), 1 + 2 * (i % 2)))

        lnA(0)
        for i in range(NT):
            if i + 1 < NT:
                lnA(i + 1)
            lnB(i)
        k.barrier()


class _V:
    def __init__(self, ap):
        self.ap = ap

    def __getitem__(self, key):
        if key == slice(None):
            return self.ap
        return self.ap[key]


def layer1(c, s):
    k, nc, di = c.k, c.nc, c.di
    XT, dXT = c.XT, c.dXT
    with contextlib.ExitStack() as ph:
        p1 = ph.enter_context(contextlib.ExitStack())
        dW = Dep()
        KD = {}
        dKD = {}
        for nm in ("s", "w"):
            for g in range(2):
                for hf in range(1):
                    KD[nm, g, hf] = k.sb("KZ%s%d%d" % (nm, g, hf), [128, S], BF16, ph)
                    k.op("pool", lambda e: e.memset(KD[nm, g, hf][64:128, :], 0.0), writes=[dW])
                dKD[nm, g] = [Dep() for _ in range(4)]
        VS = k.sb("VS", [128, NT, 2, 65], BF16, ph); dVS = [Dep() for _ in range(NT)]
        VW = k.sb("VW", [128, NT, 2, 65], BF16, ph); dVW = [Dep() for _ in range(NT)]
        GATE = k.sb("GATE", [128, NT, 48], F32, ph); dG = [Dep() for _ in range(NT)]
        KC2 = [[k.sb("KC2%d%d" % (g, hf), [128, 128], BF16, ph) for hf in range(2)] for g in range(2)]; dKC = [Dep(), Dep()]
        VCA = [k.sb("VCA%d" % g, [128, 97], BF16, ph) for g in range(2)]; dVC = [Dep(), Dep()]
        k.op("pool", lambda e: e.memset(VS[:], 1.0), writes=dVS)
        k.op("pool", lambda e: e.memset(VW[:], 1.0), writes=dVW)
        cov32 = k.sb("cov32", [128, 32], F32, ph)
        k.dma("sp", cov32[:], di["cover"][:, :], writes=[dW])
        for g in range(2):
            for hf in range(2):
                k.op("pool", lambda e: e.memset(KC2[g][hf][:], 0.0), writes=[dKC[g]])
            k.op("pool", lambda e: e.memset(VCA[g][:], 0.0), writes=[dVC[g]])
            k.op("pool", lambda e: e.memset(VCA[g][0:127, 64:65], 1.0), reads=[dVC[g]], writes=[dVC[g]])
            k.op("dve", lambda e: e.tensor_copy(out=VCA[g][:, 65:97], in_=cov32[:]), reads=[dW, dVC[g]], writes=[dVC[g]])
        Cin = k.sb("Cin", [128, 8, 1840], BF16, ph)
        dWq = Dep()
        cw = di["c_w_in"].rearrange("(kc p) n -> p kc n", p=128)
        base = {"c": 1024, "v": 1152, "s": 1280, "w": 1536}
        dCk = Dep()
        load_bf16(c, Cin[:, :, 1024:1840], cw[:, :, 1024:1840], dCk)
        Wd = {}
        ei = 0
        for nm in ("c", "v", "s", "w"):
            for g in range(2):
                Wd[nm, g] = k.sb("Wd%s%d" % (nm, g), [128, 8, 128], BF16, p1)
                for hh in range(2):
                    eng = ("dve", "act", "pool")[ei % 3]; ei += 1
                    src = Cin[:, :, base[nm] + g * 64:base[nm] + (g + 1) * 64]
                    dst = Wd[nm, g][:, :, hh * 64:(hh + 1) * 64]
                    if eng == "act":
                        k.op("act", lambda e: e.copy(out=dst, in_=src), reads=[dCk], writes=[dW])
                    else:
                        k.op(eng, lambda e: e.tensor_copy(out=dst, in_=src), reads=[dCk], writes=[dW])
        W1 = {}
        W2k = k.sb("W2k", [128, 2, 128], BF16, p1)
        W2v = k.sb("W2v", [128, 2, 64], BF16, p1)
        for kv, nm1 in (("c", "c_w_ck1"), ("v", "c_w_cv1")):
            W1[kv] = k.sb("W1" + kv, [128, 16, 256], BF16, p1)
            w1v = di[nm1].rearrange("(h l d) c -> h d l c", h=2, l=16, d=64)
            for hh in range(2):
                load_bf16(c, W1[kv][hh * 64:(hh + 1) * 64, :, :], w1v[hh], dW)
        w2kv = di["c_w_ck2"].rearrange("(cc p) d -> p cc d", p=128)
        for hh in range(2):
            load_bf16(c, W2k[:, :, hh * 64:(hh + 1) * 64], w2kv, dW)
        load_bf16(c, W2v[:], di["c_w_cv2"].rearrange("(cc p) d -> p cc d", p=128), dW)
        PE2 = k.sb("PE2", [128, 2, 16], F32, p1)
        k.dma("sp", PE2[:], di["cmp_posT"].rearrange("k p l -> p k l"), writes=[dW])
        gb = k.sb("gb", [128, 48], F32, p1)
        k.dma("sp", gb[:], di["c_gate_b"][0:1, :].partition_broadcast(128), writes=[dW])
        A2 = {}
        dA2 = {}
        for kv in ("c", "v"):
            for g in range(2):
                A2[kv, g] = k.sb("A2%s%d" % (kv, g), [128, S], BF16, p1)
                dA2[kv, g] = Dep()
                k.op("pool", lambda e: e.memset(A2[kv, g][:, S - 16:S], 0.0), writes=[dA2[kv, g]])
        load_bf16(c, Cin[:, :, 0:1024], cw[:, :, 0:1024], dWq)
        gtmp = k.sb("gtmp", [128, 48], F32, p1); dgt = Dep()
        rr = 0
        for tb in range(4):
            t0 = tb * 512
            tsl = slice(t0, t0 + 512)
            xr = dXT[tb * 4:tb * 4 + 4]
            for nm in ("c", "v", "s", "w"):
                for g in range(2):
                    pb_, dpb_ = bank(c, 4 + rr % 4); rr += 1
                    for kc in range(8):
                        k.op("pe", lambda e: e.matmul(pb_[:, 0:512], lhsT=Wd[nm, g][:, kc, :], rhs=XT[:, kc, tsl],
                                                      start=(kc == 0), stop=(kc == 7)), reads=[dW] + xr, writes=[dpb_], signal=(kc == 7))
                    if nm in ("s", "w"):
                        k.op("dve", lambda e: e.tensor_copy(out=KD[nm, g, 0][0:64, tsl], in_=pb_[0:64, 0:512]), reads=[dpb_, dW], writes=[dKD[nm, g][tb]])
                    else:
                        kvi = 0 if nm == "c" else 1
                        k.op("dve", lambda e: e.tensor_tensor(out=A2[nm, g][0:64, tsl].rearrange("p (n l) -> p n l", l=16),
                                                              in0=pb_[0:64, 0:512].rearrange("p (n l) -> p n l", l=16),
                                                              in1=PE2[0:64, kvi, :].unsqueeze(1).to_broadcast([64, 32, 16]), op=ALU.add),
                             reads=[dpb_, dW], writes=[dA2[nm, g]])
                        lo = 16 if tb == 0 else 0
                        nb = (512 - lo) // 16
                        k.op("dve", lambda e: e.tensor_tensor(out=A2[nm, g][64:128, t0 - 16 + lo:t0 + 496].rearrange("p (n l) -> p n l", l=16),
                                                              in0=pb_[64:128, lo:512].rearrange("p (n l) -> p n l", l=16),
                                                              in1=PE2[64:128, kvi, :].unsqueeze(1).to_broadcast([64, nb, 16]), op=ALU.add),
                             reads=[dpb_, dW], writes=[dA2[nm, g]])
            for ts in range(4):
                i = tb * 4 + ts
                tok = slice(i * 128, (i + 1) * 128)
                pb_, dpb_ = bank(c, rr % 4); rr += 1
                for kc in range(8):
                    k.op("pe", lambda e: e.matmul(pb_[:, 0:128], lhsT=XT[:, kc, tok], rhs=Cin[:, kc, 1408:1536], start=(kc == 0), stop=(kc == 7)),
                         reads=[dW, dCk, dXT[i]], writes=[dpb_], signal=False)
                for kc in range(8):
                    k.op("pe", lambda e: e.matmul(pb_[:, 128:304], lhsT=XT[:, kc, tok], rhs=Cin[:, kc, 1664:1840], start=(kc == 0), stop=(kc == 7)),
                         reads=[dW, dCk, dXT[i]], writes=[dpb_], signal=(kc == 7))
                k.op("act", lambda e: e.copy(out=VS[:, i, :, 0:64], in_=pb_[:, 0:128].rearrange("p (g d) -> p g d", g=2)), reads=[dpb_], writes=[dVS[i]])
                k.op("act", lambda e: e.copy(out=VW[:, i, :, 0:64], in_=pb_[:, 128:256].rearrange("p (g d) -> p g d", g=2)), reads=[dpb_], writes=[dVW[i]])
                k.op("act", lambda e: e.copy(out=gtmp[:], in_=pb_[:, 256:304]), reads=[dpb_], writes=[dgt])
                k.op("dve", lambda e: e.tensor_tensor(out=gtmp[:], in0=gtmp[:], in1=gb[:], op=ALU.add), reads=[dgt, dW], writes=[dgt])
                k.op("act", lambda e: e.activation(out=GATE[:, i, :], in_=gtmp[:], func=AF.Sigmoid), reads=[dgt], writes=[dG[i]])
        GT = k.sb("GT", [128, 2, 128], BF16, p1); dGT = Dep()
        for kv in ("c", "v"):
            for g in range(2):
                ph_, dph_ = bank(c, 0 if kv == "c" else 1)
                a2v = A2[kv, g][:, :].rearrange("p (n l) -> p n l", l=16)
                for cc in range(2):
                    for l in range(16):
                        k.op("pe", lambda e: e.matmul(ph_[:, cc * 128:cc * 128 + 127], lhsT=W1[kv][:, l, cc * 128:(cc + 1) * 128],
                                                      rhs=a2v[:, 0:127, l], start=(l == 0), stop=(l == 15)),
                             reads=[dW, dA2[kv, g]], writes=[dph_], signal=(l == 15 and cc == 1))
                k.op("act", lambda e: e.activation(out=GT[:, :, 0:127], in_=ph_[:, 0:256].rearrange("p (c n) -> p c n", c=2)[:, :, 0:127],
                                                   func=AF.Gelu_apprx_tanh), reads=[dph_], writes=[dGT])
                po_, dpo_ = bank(c, 2 if kv == "c" else 3)
                if kv == "c":
                    for cc in range(2):
                        k.op("pe", lambda e: e.matmul(po_[:, 0:127], lhsT=W2k[:, cc, :], rhs=GT[:, cc, 0:127], start=(cc == 0), stop=(cc == 1)),
                             reads=[dW, dGT], writes=[dpo_], signal=(cc == 1))
                    k.op("act", lambda e: e.copy(out=KC2[g][0][0:64, 0:127], in_=po_[0:64, 0:127]), reads=[dpo_, dKC[g]], writes=[dKC[g]])
                    k.op("act", lambda e: e.copy(out=KC2[g][1][64:128, 0:127], in_=po_[64:128, 0:127]), reads=[dpo_, dKC[g]], writes=[dKC[g]])
                else:
                    for cc in range(2):
                        k.op("pe", lambda e: e.matmul(po_[0:127, 0:64], lhsT=GT[:, cc, 0:127], rhs=W2v[:, cc, :], start=(cc == 0), stop=(cc == 1)),
                             reads=[dW, dGT], writes=[dpo_], signal=(cc == 1))
                    k.op("act", lambda e: e.copy(out=VCA[g][0:127, 0:64], in_=po_[0:127, 0:64]), reads=[dpo_, dVC[g]], writes=[dVC[g]])
        k.barrier()
        p1.close()
        cm32 = k.sb("cm32", [128, S], F32, ph)
        cmask = k.sb("cmask", [128, S], BF16, ph)
        dC = Dep()
        k.dma("sp", cm32[:], di["cmask"][:, :], writes=[dC])
        k.op("pool", lambda e: e.tensor_scalar(out=cmask[:], in0=cm32[:], scalar1=-1.0, scalar2=30000.0, op0=ALU.add, op1=ALU.mult),
             reads=[dC], writes=[dC])
        cand = k.sb("cand", [128, NT * 32], F32, ph); sbias = k.sb("sbias", [128, NT * 32], F32, ph)
        k.dma("sp", cand[:], di["cand"][:, :], writes=[dC])
        k.dma("sp", sbias[:], di["sbias"][:, :], writes=[dC])
        for g in range(2):
            c.k.dma("pool", KD["s", g, 0][64:96, :], di["e2"][:, :], writes=dKD["s", g])
        Qe = k.sb("Qe", [128, 8, 512], BF16, ph); Qo = k.sb("Qo", [128, 8, 512], BF16, ph)
        QQ = (Qe, Qo)
        dQq = Dep(); dQs = [Dep(), Dep()]
        k.op("pool", lambda e: e.memset(Qe[64:128, :, :], 0.0), writes=dQs)
        k.op("pool", lambda e: e.memset(Qo[64:128, :, :], 0.0), writes=dQs)
        qrr = [0]
        O = k.sb("O", [128, 4, D], F32, ph); dO = [Dep() for _ in range(4)]
        PT = [k.sb("PT%d" % j, [128, 512], BF16, ph) for j in range(6)]; dPT = [Dep() for _ in range(6)]
        imp = k.sb("imp", [128, 4, 32], F32, ph); dimp = [Dep() for _ in range(4)]
        sm = k.sb("sm1", [128, 64], F32, ph); dsm = Dep()
        otmp = k.sb("otmp", [128, 4, 64], F32, ph); dot = Dep()
        selb = k.sb("selb", [128, 32], BF16, ph)
        for tb in range(4):
            tsl = slice(tb * 512, (tb + 1) * 512)
            for cq in range(8):
                for par in range(2):
                    pb_, dpb_ = bank(c, 4 + qrr[0] % 4); qrr[0] += 1
                    for kc in range(8):
                        k.op("pe", lambda e: e.matmul(pb_[0:64, 0:512], lhsT=Cin[:, kc, cq * 128 + par * 64:cq * 128 + par * 64 + 64], rhs=XT[:, kc, tsl],
                                                      start=(kc == 0), stop=(kc == 7)), reads=[dWq] + dXT[tb * 4:tb * 4 + 4], writes=[dpb_], signal=(kc == 7))
                    k.op("act", lambda e: e.activation(out=QQ[par][0:64, cq, :], in_=pb_[0:64, 0:512], func=AF.Copy, scale=0.125),
                         reads=[dpb_], writes=[dQq])
            for g in range(2):
                def q_ap(h, rows, t0, t1):
                    H = g * 8 + h
                    return QQ[H % 2][0:rows, H // 2, t0 - tb * 512:t1 - tb * 512]

                def fin_gen(br, first):
                    def fin(hg, ts, acc, dacc):
                        i = tb * 4 + ts
                        den, rden, coef = sm[:, 0:4], sm[:, 4:8], sm[:, 8:12]
                        k.op("dve", lambda e: e.tensor_scalar(out=den, in0=acc[:, :, 64], scalar1=1e-30, scalar2=None, op0=ALU.max),
                             reads=[dacc, dsm], writes=[dsm])
                        k.op("dve", lambda e: e.reciprocal(out=rden, in_=den), reads=[dsm], writes=[dsm])
                        g0 = br * 16 + g * 8 + hg * 4
                        k.op("dve", lambda e: e.tensor_tensor(out=coef, in0=rden, in1=GATE[:, i, g0:g0 + 4], op=ALU.mult),
                             reads=[dsm, dG[i]], writes=[dsm])
                        c0 = (g * 8 + hg * 4) * 64
                        Ov = O[:, ts, c0:c0 + 256].rearrange("p (h d) -> p h d", h=4)
                        cb = coef.unsqueeze(2).to_broadcast([128, 4, 64])
                        if first:
                            k.op("dve", lambda e: e.tensor_tensor(out=Ov, in0=acc[:, :, 0:64], in1=cb, op=ALU.mult),
                                 reads=[dacc, dsm], writes=[dO[ts]])
                            for h in range(4):
                                if hg == 0 and h == 0:
                                    k.op("dve", lambda e: e.tensor_scalar(out=imp[:, ts, :], in0=acc[:, h, 65:97], scalar1=rden[:, h:h + 1],
                                                                          scalar2=None, op0=ALU.mult), reads=[dacc, dsm], writes=[dimp[ts]])
                                else:
                                    k.op("dve", lambda e: e.scalar_tensor_tensor(out=imp[:, ts, :], in0=acc[:, h, 65:97], scalar=rden[:, h:h + 1],
                                                                                 in1=imp[:, ts, :], op0=ALU.mult, op1=ALU.add),
                                         reads=[dacc, dsm, dimp[ts]], writes=[dimp[ts]])
                        else:
                            k.op("dve", lambda e: e.tensor_tensor(out=otmp[:], in0=acc[:, :, 0:64], in1=cb, op=ALU.mult),
                                 reads=[dacc, dsm], writes=[dot])
                            k.op("pool", lambda e: e.tensor_tensor(out=Ov, in0=Ov, in1=otmp[:], op=ALU.add), reads=[dot, dO[ts]], writes=[dO[ts]])
                    return fin

                def mask_cmp(h, kt, lo, hi):
                    return [(0, 512, c.identb[:], cmask[:, tb * 512:(tb + 1) * 512], [dC, c.dconst])]
                attention(c, ph, 8, 4, (lambda h, kt, t0, t1: (KC2[g][0][0:64, :], q_ap(h, 64, t0, t1), [dKC[g], dQq])),
                          lambda tb_: [0], lambda tb_, kt: (0, 3), mask_cmp, lambda h, kt: (VCA[g][:, :], [dVC[g]]), 97,
                          fin_gen(0, True), tb, PT, dPT, acc_banks=(0, 1, 2, 3), s_banks=(4, 5, 6))
                for ts in range(4):
                    i = tb * 4 + ts
                    sc, top8, selm_ = sm[:, 16:48], sm[:, 48:56], sm[:, 16:48]
                    k.op("dve", lambda e: e.tensor_tensor(out=sc, in0=imp[:, ts, :], in1=cand[:, i * 32:(i + 1) * 32], op=ALU.mult),
                         reads=[dimp[ts], dC, dsm], writes=[dsm])
                    k.op("dve", lambda e: e.tensor_tensor(out=sc, in0=sc, in1=sbias[:, i * 32:(i + 1) * 32], op=ALU.add), reads=[dsm, dC], writes=[dsm])
                    k.op("dve", lambda e: e.max(out=top8, in_=sc), reads=[dsm], writes=[dsm])
                    k.op("dve", lambda e: e.tensor_scalar(out=selb[:], in0=sc, scalar1=top8[:, 7:8], scalar2=None, op0=ALU.is_ge),
                         reads=[dsm], writes=[dsm])
                    k.op("dve", lambda e: e.tensor_scalar(out=selb[:], in0=selb[:], scalar1=-1.0, scalar2=30000.0, op0=ALU.add, op1=ALU.mult),
                         reads=[dsm], writes=[dsm])
                    pt_, dpt_ = bank(c, 7)
                    ptb = pt_[:].bitcast(BF16)
                    k.op("pe", lambda e: e.transpose(out=ptb[64:96, 0:128], in_=selb[:, :], identity=c.identb[:]), reads=[dsm, c.dconst], writes=[dpt_])
                    for par in range(2):
                        k.op("act", lambda e: e.copy(out=QQ[par][64:96, g * 4:(g + 1) * 4, ts * 128:(ts + 1) * 128],
                                                     in_=ptb[64:96, 0:128].unsqueeze(1).to_broadcast([32, 4, 128])), reads=[dpt_], writes=[dQs[g]])

                def mask_sel(h, kt, lo, hi):
                    c0 = lo * 128
                    if kt >= 4 * tb:
                        return [(c0, c0 + 128, c.identb[:], c.ntri[:], [c.dconst])]
                    return []
                attention(c, ph, 8, 4, (lambda h, kt, t0, t1: (KD["s", g, 0][0:96, kt * 128:(kt + 1) * 128], q_ap(h, 96, t0, t1),
                                                              [dKD["s", g][kt // 4], dQq, dQs[g]])),
                          lambda tb_: list(range(0, 4 * tb_ + 4)), lambda tb_, kt: (max(0, kt - 4 * tb_), 3), mask_sel,
                          lambda h, kt: (VS[:, kt, g, :], [dVS[kt]]), 65, fin_gen(1, False), tb, PT, dPT, acc_banks=(0, 1, 2, 3), s_banks=(4, 5, 6))

                def mask_win(h, kt, lo, hi):
                    res = []
                    for ts in range(lo, hi + 1):
                        dl = 4 * tb + ts - kt
                        if dl == 0:
                            res.append((ts * 128, ts * 128 + 128, c.identb[:], c.ntri[:], [c.dconst]))
                        elif dl == 4:
                            res.append((ts * 128, ts * 128 + 128, c.identb[:], c.ntri2[:], [c.dconst]))
                    return res
                attention(c, ph, 8, 4, (lambda h, kt, t0, t1: (KD["w", g, 0][:, kt * 128:(kt + 1) * 128], q_ap(h, 128, t0, t1),
                                                              [dKD["w", g][kt // 4], dQq, dQs[g]])),
                          lambda tb_: list(range(max(0, 4 * tb_ - 4), 4 * tb_ + 4)),
                          lambda tb_, kt: (max(0, kt - 4 * tb_), min(3, kt - 4 * tb_ + 4)), mask_win,
                          lambda h, kt: (VW[:, kt, g, :], [dVW[kt]]), 65, fin_gen(2, False), tb, PT, dPT, acc_banks=(0, 1, 2, 3), s_banks=(4, 5, 6))
            for ts in range(4):
                i = tb * 4 + ts
                transpose_to_XT(c, _V(O[:, ts, :]), dO[ts], i, banks=(6, 7))
        k.barrier()


def host_consts():
    h = {}
    h["ident"] = np.eye(128, dtype=np.float32)
    kk = np.arange(128)[:, None]; tt = np.arange(128)[None, :]
    h["tri"] = (kk <= tt).astype(np.float32)
    h["tri2"] = (kk > tt).astype(np.float32)
    fr = np.exp(-math.log(10000.0) * np.arange(16, dtype=np.float32) / 16).astype(np.float32)
    h["freq"] = np.tile((fr / np.float32(2 * math.pi)).astype(np.float32)[None, :], (128, 1))
    sel = np.zeros((32, 32, 128), np.float32)
    for e in range(32):
        sel[e, e, :] = 1.0
    h["selm"] = sel.reshape(32, 32 * 128)
    n = np.arange(128)[:, None]; t = np.arange(S)[None, :]
    h["cmask"] = ((t >= 16 * n + 31) & (n < 127)).astype(np.float32)
    c0 = np.arange(128)[:, None] * 16; s0 = np.arange(32)[None, :] * 64
    h["cover"] = ((c0 < s0 + 64) & (c0 + 32 > s0) & (np.arange(128)[:, None] < 127)).astype(np.float32)
    tok = np.arange(S); tb = tok // 64; jj = np.arange(32)[None, :]
    forced = (jj == 0) | (jj == tb[:, None]) | (jj == tb[:, None] - 1)
    cand = (~forced) & (jj <= tb[:, None])
    sbias = np.where(forced, 1e4, np.where(jj <= tb[:, None], 0.0, -1e4)).astype(np.float32)
    h["cand"] = cand.astype(np.float32).reshape(NT, 128, 32).transpose(1, 0, 2).reshape(128, NT * 32).copy()
    h["sbias"] = sbias.reshape(NT, 128, 32).transpose(1, 0, 2).reshape(128, NT * 32).copy()
    e2 = np.zeros((32, NT, 128), np.float32)
    for kt in range(NT):
        for m in range(128):
            e2[2 * kt + m // 64, kt, m] = 1.0
    h["e2"] = e2.reshape(32, NT * 128)
    return h


def host_inputs(inputs, seqs):
    f = lambda a: np.ascontiguousarray(np.asarray(a, dtype=np.float32))
    m = {}
    m["x"] = f(inputs["x"][seqs])
    pos = np.asarray(inputs["positions"])[seqs].astype(np.int32)
    m["posT"] = np.ascontiguousarray(pos.reshape(len(seqs), NT, 128).transpose(2, 0, 1).reshape(128, len(seqs) * NT))
    m["ab_w_in"] = f(inputs["ab_w_in"][0]); m["gm_ln_g"] = f(inputs["ab_gm_ln_g"]); m["gm_ln_b"] = f(inputs["ab_gm_ln_b"])
    m["gm_wsT"] = f(np.asarray(inputs["ab_gm_ws"][0]).transpose(0, 2, 1)); m["gm_bs"] = f(np.asarray(inputs["ab_gm_bs"][0]).reshape(1, 512))
    m["qg2"] = f(np.asarray(inputs["ab_mla_q_norm"][0]).reshape(2, 128).T); m["kvg"] = f(np.asarray(inputs["ab_mla_kv_norm"][0]).reshape(128, 1))
    m["w_uq"] = f(inputs["ab_mla_w_uq"][0]); m["w_uk"] = f(inputs["ab_mla_w_uk"][0]); m["w_uv"] = f(inputs["ab_mla_w_uv"][0])
    m["ab_w_o"] = f(inputs["ab_w_o"][0]); m["c_w_in"] = f(inputs["c_w_in"][0])
    cp = np.asarray(inputs["c_cmp_pos"][0])
    m["cmp_posT"] = f(cp.reshape(2, 2, 16, 64).transpose(0, 1, 3, 2).reshape(2, 128, 16))
    for n_ in ("c_w_ck1", "c_w_ck2", "c_w_cv1", "c_w_cv2", "c_w_o"):
        m[n_] = f(inputs[n_][0])
    m["c_gate_b"] = f(np.asarray(inputs["c_gate_b"]).reshape(1, 48))
    for n_ in ("moe_w_rg", "moe_b_rg", "moe_w_re", "moe_b_re", "moe_w_gate", "moe_w_up", "moe_w_down", "ln1_g", "ln1_b", "ln2_g", "ln2_b"):
        m[n_] = f(inputs[n_])
    m.update(host_consts())
    return m


_CACHE = {}


def kernel(**inputs):
    ncores, nseq = 8, 2
    if "nc" not in _CACHE:
        _CACHE["nc"] = build(nseq)[0]
    nc = _CACHE["nc"]
    base = host_inputs(inputs, [0, 1])
    in_maps = []
    x = np.asarray(inputs["x"], dtype=np.float32)
    pos = np.asarray(inputs["positions"]).astype(np.int32)
    for cid in range(ncores):
        m = dict(base)
        seqs = [2 * cid, 2 * cid + 1]
        m["x"] = np.ascontiguousarray(x[seqs])
        m["posT"] = np.ascontiguousarray(pos[seqs].reshape(nseq, NT, 128).transpose(2, 0, 1).reshape(128, nseq * NT))
        in_maps.append(m)
    res = run_bass_kernel_spmd(nc, in_maps, core_ids=list(range(ncores)))
    out = np.concatenate([np.asarray(r["out"]) for r in res.results], axis=0)
    return out.astype(np.float32)
```
